# Optimizing a Trainium2 kernel written in Bass

```python
import math
import jax, jax.numpy as jnp
from jax import lax
import numpy as np

D_MODEL = 2048
BATCH = 1
SEQ = 8192
DEPTH = 2

N_MEM = 256
S5_WIDTH = 1024
S5_GROUP = 16
S5_GROUPS = S5_WIDTH // S5_GROUP
S5_STATE = 64
S5_DT_MIN = 1e-3
S5_DT_MAX = 1e-1
MLA_HEADS = 16
MLA_Q_RANK = 448
MLA_KV_RANK = 512
MLA_NOPE = 128
MLA_ROPE = 64
MLA_QK = MLA_NOPE + MLA_ROPE
MLA_V = 128
ROPE_THETA = 10000.0
Q_BLOCK = 128
MEM_HEADS = 4
MEM_HEAD_DIM = 256
MEM_WIDTH = MEM_HEADS * MEM_HEAD_DIM
N_BRANCH = 3
S5_END = S5_WIDTH
CQ_END = S5_END + MLA_Q_RANK
CKV_END = CQ_END + MLA_KV_RANK
KR_END = CKV_END + MLA_ROPE
MQ_END = KR_END + MEM_WIDTH
IN_COLS = MQ_END + N_BRANCH * D_MODEL
D_FF = 7168
N_EXPERTS = 8
TOP_K = 2
D_FF_EXPERT = 7168
N_DENSE = (DEPTH + 1) // 2
N_MOE = DEPTH // 2
EPS = 1e-6
NEG_INF = -1e30

kernel_name = 'hybrid_s5_mla_memory_moe_trunk'


def rms_norm(x, g):
    xf = x.astype(jnp.float32)
    y = xf * lax.rsqrt(jnp.mean(xf * xf, axis=-1, keepdims=True) + EPS)
    return (y * g.astype(jnp.float32)).astype(x.dtype)


def rope_tables(positions):
    inv_freq = ROPE_THETA ** (-jnp.arange(0, MLA_ROPE, 2, dtype=jnp.float32) / MLA_ROPE)
    ang = positions.astype(jnp.float32)[..., None] * inv_freq
    return jnp.cos(ang)[:, :, None, :], jnp.sin(ang)[:, :, None, :]


def rope_tail(x, cos, sin):
    x_n, x_r = jnp.split(x, [MLA_NOPE], axis=-1)
    xr = x_r.astype(jnp.float32)
    x1, x2 = jnp.split(xr, 2, axis=-1)
    rot = jnp.concatenate([x1 * cos - x2 * sin, x2 * cos + x1 * sin], axis=-1)
    return jnp.concatenate([x_n, rot.astype(x.dtype)], axis=-1)


def complex_linear_combine(e1, e2):
    a1r, a1i, b1r, b1i = e1
    a2r, a2i, b2r, b2i = e2
    return (a2r * a1r - a2i * a1i,
            a2r * a1i + a2i * a1r,
            a2r * b1r - a2i * b1i + b2r,
            a2r * b1i + a2i * b1r + b2i)


def s5_branch(u, a_re, a_im, log_dt, b_re, b_im, c_re, c_im, d, w_glu):
    bsz, L, _ = u.shape
    f32 = jnp.float32
    uf = u.astype(f32).reshape(bsz, L, S5_GROUPS, S5_GROUP)
    a_re = a_re.astype(f32)
    a_im = a_im.astype(f32)
    b_re = b_re.astype(f32)
    b_im = b_im.astype(f32)
    dt = jnp.exp(log_dt.astype(f32))[:, None]
    mag = jnp.exp(dt * a_re)
    lam_re = mag * jnp.cos(dt * a_im)
    lam_im = mag * jnp.sin(dt * a_im)
    den = a_re * a_re + a_im * a_im
    n_re = lam_re - 1.0
    n_im = lam_im
    f_re = ((n_re * a_re + n_im * a_im) / den)[..., None]
    f_im = ((n_im * a_re - n_re * a_im) / den)[..., None]
    bb_re = f_re * b_re - f_im * b_im
    bb_im = f_re * b_im + f_im * b_re
    bu_re = jnp.einsum('gph,blgh->blgp', bb_re, uf)
    bu_im = jnp.einsum('gph,blgh->blgp', bb_im, uf)
    lr = jnp.broadcast_to(lam_re, bu_re.shape)
    li = jnp.broadcast_to(lam_im, bu_re.shape)
    _, _, s_re, s_im = lax.associative_scan(complex_linear_combine, (lr, li, bu_re, bu_im), axis=1)
    y = (jnp.einsum('ghp,blgp->blgh', c_re.astype(f32), s_re)
         - jnp.einsum('ghp,blgp->blgh', c_im.astype(f32), s_im)
         + d.astype(f32) * uf)
    y = jax.nn.gelu(y.reshape(bsz, L, S5_WIDTH)).astype(u.dtype)
    ga, gb = jnp.split(y @ w_glu, 2, axis=-1)
    return ga * jax.nn.sigmoid(gb)


def causal_block_attention(q, k, v, positions):
    bsz, L, H, dq = q.shape
    nb = L // Q_BLOCK
    scale = dq ** -0.5
    qb = q.reshape(bsz, nb, Q_BLOCK, H, dq).transpose(1, 0, 2, 3, 4)
    pb = positions.reshape(bsz, nb, Q_BLOCK).transpose(1, 0, 2)

    def one_block(args):
        q_blk, p_blk = args
        s = jnp.einsum('bqhd,bkhd->bhqk', q_blk, k).astype(jnp.float32) * scale
        mask = positions[:, None, None, :] <= p_blk[:, None, :, None]
        s = jnp.where(mask, s, NEG_INF)
        p = jax.nn.softmax(s, axis=-1).astype(v.dtype)
        return jnp.einsum('bhqk,bkhd->bqhd', p, v)

    o = lax.map(one_block, (qb, pb))
    return o.transpose(1, 0, 2, 3, 4).reshape(bsz, L, H, v.shape[-1])


def mla_branch(c_q, c_kv, k_rope, positions, cos, sin, q_a_norm, w_q_b, kv_norm,
               w_kv_b, q_norm, k_norm, w_o):
    bsz, L, _ = c_q.shape
    q = (rms_norm(c_q, q_a_norm) @ w_q_b).reshape(bsz, L, MLA_HEADS, MLA_QK)
    kv = (rms_norm(c_kv, kv_norm) @ w_kv_b).reshape(bsz, L, MLA_HEADS, MLA_NOPE + MLA_V)
    k_nope, v = jnp.split(kv, [MLA_NOPE], axis=-1)
    k_r = jnp.broadcast_to(k_rope[:, :, None, :], (bsz, L, MLA_HEADS, MLA_ROPE))
    k = jnp.concatenate([k_nope, k_r], axis=-1)
    q = rope_tail(rms_norm(q, q_norm), cos, sin)
    k = rope_tail(rms_norm(k, k_norm), cos, sin)
    o = causal_block_attention(q, k, v, positions)
    return o.reshape(bsz, L, MLA_HEADS * MLA_V) @ w_o


def memory_branch(q_mem, mem, mem_norm_g, w_kv, q_norm, k_norm, w_o):
    bsz, L, _ = q_mem.shape
    m_len = mem.shape[1]
    m = rms_norm(mem, mem_norm_g)
    k, v = jnp.split(m @ w_kv, 2, axis=-1)
    k = rms_norm(k.reshape(bsz, m_len, MEM_HEADS, MEM_HEAD_DIM), k_norm)
    v = v.reshape(bsz, m_len, MEM_HEADS, MEM_HEAD_DIM)
    q = rms_norm(q_mem.reshape(bsz, L, MEM_HEADS, MEM_HEAD_DIM), q_norm)
    s = jnp.einsum('blhd,bmhd->bhlm', q, k).astype(jnp.float32) * (MEM_HEAD_DIM ** -0.5)
    p = jax.nn.softmax(s, axis=-1).astype(v.dtype)
    o = jnp.einsum('bhlm,bmhd->blhd', p, v).reshape(bsz, L, MEM_WIDTH)
    return o @ w_o


def swiglu(h, w_gate_up, w_down):
    g, u = jnp.split(h @ w_gate_up, 2, axis=-1)
    return (jax.nn.silu(g) * u) @ w_down


def moe_ffn(h, router, router_b, w_gate_up, w_down):
    bsz, L, D = h.shape
    t = h.reshape(bsz * L, D)
    logits = (t @ router).astype(jnp.float32) + router_b.astype(jnp.float32)
    top_v, top_i = lax.top_k(logits, TOP_K)
    top_w = jax.nn.softmax(top_v, axis=-1)
    out = jnp.zeros_like(t)
    for e in range(N_EXPERTS):
        w_e = jnp.sum(jnp.where(top_i == e, top_w, 0.0), axis=-1).astype(t.dtype)
        out = out + w_e[:, None] * swiglu(t, w_gate_up[e], w_down[e])
    return out.reshape(bsz, L, D)


def setup_inputs(seed: int = 0) -> dict:
    key = jax.random.key(seed)
    ks = iter(jax.random.split(key, 40))
    f32 = jnp.float32

    def nrm(shape, fan_in):
        return jax.random.normal(next(ks), shape, f32) * (fan_in ** -0.5)

    def gain(shape):
        return 1.0 + 0.02 * jax.random.normal(next(ks), shape, f32)

    x = jax.random.normal(next(ks), (BATCH, SEQ, D_MODEL), f32)
    mem = jax.random.normal(next(ks), (BATCH, N_MEM, D_MODEL), f32)
    offset = jax.random.randint(next(ks), (BATCH, 1), 0, 4096, jnp.int32)
    positions = offset + jnp.arange(SEQ, dtype=jnp.int32)[None, :]
    n_idx = jnp.arange(S5_STATE, dtype=f32)
    gp = (DEPTH, S5_GROUPS, S5_STATE)
    return {
        'x': x,
        'mem': mem,
        'positions': positions,
        'norm_mix': gain((DEPTH, D_MODEL)),
        'w_in': nrm((DEPTH, D_MODEL, IN_COLS), D_MODEL),
        's5_a_re': -0.5 + 0.01 * jax.random.normal(next(ks), gp, f32),
        's5_a_im': math.pi * n_idx + 0.01 * jax.random.normal(next(ks), gp, f32),
        's5_log_dt': jax.random.uniform(next(ks), (DEPTH, S5_GROUPS), f32,
                                        math.log(S5_DT_MIN), math.log(S5_DT_MAX)),
        's5_b_re': nrm((DEPTH, S5_GROUPS, S5_STATE, S5_GROUP), 2 * S5_GROUP),
        's5_b_im': nrm((DEPTH, S5_GROUPS, S5_STATE, S5_GROUP), 2 * S5_GROUP),
        's5_c_re': nrm((DEPTH, S5_GROUPS, S5_GROUP, S5_STATE), S5_STATE),
        's5_c_im': nrm((DEPTH, S5_GROUPS, S5_GROUP, S5_STATE), S5_STATE),
        's5_d': jax.random.normal(next(ks), (DEPTH, S5_GROUPS, S5_GROUP), f32),
        's5_w_glu': nrm((DEPTH, S5_WIDTH, 2 * D_MODEL), S5_WIDTH),
        'mla_q_a_norm': gain((DEPTH, MLA_Q_RANK)),
        'mla_w_q_b': nrm((DEPTH, MLA_Q_RANK, MLA_HEADS * MLA_QK), MLA_Q_RANK),
        'mla_kv_norm': gain((DEPTH, MLA_KV_RANK)),
        'mla_w_kv_b': nrm((DEPTH, MLA_KV_RANK, MLA_HEADS * (MLA_NOPE + MLA_V)), MLA_KV_RANK),
        'mla_q_norm': gain((DEPTH, MLA_QK)),
        'mla_k_norm': gain((DEPTH, MLA_QK)),
        'mla_w_o': nrm((DEPTH, MLA_HEADS * MLA_V, D_MODEL), MLA_HEADS * MLA_V),
        'mem_norm': gain((DEPTH, D_MODEL)),
        'mem_w_kv': nrm((DEPTH, D_MODEL, 2 * MEM_WIDTH), D_MODEL),
        'mem_q_norm': gain((DEPTH, MEM_HEAD_DIM)),
        'mem_k_norm': gain((DEPTH, MEM_HEAD_DIM)),
        'mem_w_o': nrm((DEPTH, MEM_WIDTH, D_MODEL), MEM_WIDTH),
        'w_out': nrm((DEPTH, D_MODEL, D_MODEL), D_MODEL),
        'norm_ffn': gain((DEPTH, D_MODEL)),
        'ffn_w_gate_up': nrm((N_DENSE, D_MODEL, 2 * D_FF), D_MODEL),
        'ffn_w_down': nrm((N_DENSE, D_FF, D_MODEL), D_FF),
        'moe_router': nrm((N_MOE, D_MODEL, N_EXPERTS), D_MODEL),
        'moe_router_b': 0.01 * jax.random.normal(next(ks), (N_MOE, N_EXPERTS), f32),
        'moe_w_gate_up': nrm((N_MOE, N_EXPERTS, D_MODEL, 2 * D_FF_EXPERT), D_MODEL),
        'moe_w_down': nrm((N_MOE, N_EXPERTS, D_FF_EXPERT, D_MODEL), D_FF_EXPERT),
    }


def reference(x, mem, positions, norm_mix, w_in, s5_a_re, s5_a_im, s5_log_dt, s5_b_re,
              s5_b_im, s5_c_re, s5_c_im, s5_d, s5_w_glu, mla_q_a_norm, mla_w_q_b,
              mla_kv_norm, mla_w_kv_b, mla_q_norm, mla_k_norm, mla_w_o, mem_norm,
              mem_w_kv, mem_q_norm, mem_k_norm, mem_w_o, w_out, norm_ffn,
              ffn_w_gate_up, ffn_w_down, moe_router, moe_router_b, moe_w_gate_up,
              moe_w_down):
    bsz, L, D = x.shape
    cos, sin = rope_tables(positions)
    for l in range(DEPTH):
        h = rms_norm(x, norm_mix[l])
        proj = h @ w_in[l]
        u, c_q, c_kv, k_rope, q_mem, gate_logits = jnp.split(
            proj, [S5_END, CQ_END, CKV_END, KR_END, MQ_END], axis=-1)
        y_s5 = s5_branch(u, s5_a_re[l], s5_a_im[l], s5_log_dt[l], s5_b_re[l], s5_b_im[l],
                         s5_c_re[l], s5_c_im[l], s5_d[l], s5_w_glu[l])
        y_mla = mla_branch(c_q, c_kv, k_rope, positions, cos, sin, mla_q_a_norm[l],
                           mla_w_q_b[l], mla_kv_norm[l], mla_w_kv_b[l], mla_q_norm[l],
                           mla_k_norm[l], mla_w_o[l])
        y_mem = memory_branch(q_mem, mem, mem_norm[l], mem_w_kv[l], mem_q_norm[l],
                              mem_k_norm[l], mem_w_o[l])
        g = jax.nn.sigmoid(gate_logits.astype(jnp.float32)).astype(x.dtype)
        g = g.reshape(bsz, L, N_BRANCH, D)
        merged = g[:, :, 0, :] * y_s5 + g[:, :, 1, :] * y_mla + g[:, :, 2, :] * y_mem
        x = x + merged @ w_out[l]
        h = rms_norm(x, norm_ffn[l])
        if l % 2 == 0:
            x = x + swiglu(h, ffn_w_gate_up[l // 2], ffn_w_down[l // 2])
        else:
            x = x + moe_ffn(h, moe_router[l // 2], moe_router_b[l // 2],
                            moe_w_gate_up[l // 2], moe_w_down[l // 2])
    return x
```

```python
import numpy as np
from contextlib import ExitStack
import concourse.bass as bass
import concourse.mybir as mybir
from concourse.bass_utils import run_bass_kernel_spmd

F32 = mybir.dt.float32
BF16 = mybir.dt.bfloat16
I32 = mybir.dt.int32
AF = mybir.ActivationFunctionType
ALU = mybir.AluOpType
AX = mybir.AxisListType

ENGS = ["pe", "act", "dve", "pool", "sp"]
NDMASEM = 6


class Buf:
    __slots__ = ("name", "writer", "readers")

    def __init__(self, name=""):
        self.name = name
        self.writer = {}
        self.readers = {}


class Prog:
    def __init__(self, nc):
        self.nc = nc
        self.es = ExitStack()
        self.streams = {e: [] for e in ENGS}
        self.sems = {}
        self.cnt = {}
        for e in ["pe", "act", "dve", "pool"]:
            self.sems[e] = self.es.enter_context(nc.semaphore("s_" + e))
            self.cnt[e] = 0
        self.dsem = {}
        self.dcnt = {}
        self.dn = {}
        for q in ["sp", "act", "pool"]:
            self.dsem[q] = [self.es.enter_context(nc.semaphore(f"d_{q}{i}")) for i in range(NDMASEM)]
            self.dcnt[q] = [0] * NDMASEM
            self.dn[q] = 0
        self.waited = {e: {} for e in ENGS}
        self.arena_words = 51200
        self.arena = self.es.enter_context(nc.sbuf_tensor("arena", [128, self.arena_words], F32))
        self.apos = 0
        self.psum = [self.es.enter_context(nc.psum_tensor(f"ps{i}", [128, 512], F32)) for i in range(8)]
        self.psb = [Buf(f"ps{i}") for i in range(8)]
        self.psi = 0
        self.ps_pool = list(range(8))
        self.all_bufs = []

    def alloc(self, words, name=""):
        off = self.apos
        self.apos += words
        assert self.apos <= self.arena_words, f"SBUF arena overflow {self.apos}"
        return self.arena[:, off:off + words]

    def alloc_f32(self, n, name=""):
        return self.alloc(n, name)

    def alloc_bf16(self, n, name=""):
        assert n % 2 == 0
        return self.alloc(n // 2, name).bitcast(BF16)

    def mark(self):
        return self.apos

    def release(self, mark):
        self.barrier_all()
        self.apos = mark

    def buf(self, name=""):
        b = Buf(name)
        self.all_bufs.append(b)
        return b

    def _add_wait(self, eng, waits, t):
        if t is None:
            return
        key, val = t
        if self.waited[eng].get(key, 0) >= val:
            return
        if key == ("e", eng) and val > self.cnt[eng]:
            return
        for i, (k, v) in enumerate(waits):
            if k == key:
                if v < val:
                    waits[i] = (key, val)
                return
        waits.append((key, val))

    def op(self, eng, fn, reads=(), writes=(), signal=True, extra_waits=()):
        waits = []
        for b in reads:
            for r in b.writer.items():
                self._add_wait(eng, waits, r)
        for b in writes:
            for r in b.writer.items():
                self._add_wait(eng, waits, r)
            for r in b.readers.items():
                self._add_wait(eng, waits, r)
        for t in extra_waits:
            self._add_wait(eng, waits, t)
        for k, v in waits:
            self.waited[eng][k] = max(self.waited[eng].get(k, 0), v)
        if signal:
            self.cnt[eng] += 1
            tick = (("e", eng), self.cnt[eng])
        else:
            tick = (("e", eng), self.cnt[eng] + 1)
        self.streams[eng].append((waits, fn, ("e", eng) if signal else None))
        for b in reads:
            b.readers[tick[0]] = max(b.readers.get(tick[0], 0), tick[1])
        for b in writes:
            b.writer[tick[0]] = max(b.writer.get(tick[0], 0), tick[1])
            b.readers = {}
        return tick

    def dma(self, q, fn, reads=(), writes=(), extra_waits=()):
        eng = {"sp": "sp", "act": "act", "pool": "pool"}[q]
        waits = []
        for b in reads:
            for r in b.writer.items():
                self._add_wait(eng, waits, r)
        for b in writes:
            for r in b.writer.items():
                self._add_wait(eng, waits, r)
            for r in b.readers.items():
                self._add_wait(eng, waits, r)
        for t in extra_waits:
            self._add_wait(eng, waits, t)
        n = self.dn[q]
        slot = n % NDMASEM
        if self.dcnt[q][slot] > 0:
            self._add_wait(eng, waits, (("d", q, slot), self.dcnt[q][slot]))
        for k, v in waits:
            self.waited[eng][k] = max(self.waited[eng].get(k, 0), v)
        self.dn[q] += 1
        self.dcnt[q][slot] += 16
        tick = (("d", q, slot), self.dcnt[q][slot])
        self.streams[eng].append((waits, fn, ("d", q, slot)))
        for b in reads:
            b.readers[tick[0]] = max(b.readers.get(tick[0], 0), tick[1])
        for b in writes:
            b.writer[tick[0]] = max(b.writer.get(tick[0], 0), tick[1])
            b.readers = {}
        return tick

    def sem_of(self, key):
        if key[0] == "e":
            return self.sems[key[1]]
        return self.dsem[key[1]][key[2]]

    def barrier_all(self):
        ticks = []
        for e in ["pe", "act", "dve", "pool"]:
            if self.cnt[e] > 0:
                ticks.append((("e", e), self.cnt[e]))
        for q in ["sp", "act", "pool"]:
            for s in range(NDMASEM):
                if self.dcnt[q][s] > 0:
                    ticks.append((("d", q, s), self.dcnt[q][s]))
        for e in ENGS:
            waits = []
            for t in ticks:
                self._add_wait(e, waits, t)
            for k, v in waits:
                self.waited[e][k] = max(self.waited[e].get(k, 0), v)
            if waits:
                self.streams[e].append((waits, None, None))
        for b in self.all_bufs:
            b.writer = {}
            b.readers = {}
        for b in self.psb:
            b.writer = {}
            b.readers = {}

    def finish(self):
        self.barrier_all()
        nc = self.nc
        P = self

        def replay(ename):
            def f(e):
                for waits, fn, sig in P.streams[ename]:
                    for k, v in waits:
                        e.wait_ge(P.sem_of(k), v)
                    if fn is None:
                        continue
                    ins = fn(e)
                    if sig is not None:
                        if sig[0] == "e":
                            ins.then_inc(P.sems[sig[1]], 1)
                        else:
                            ins.then_inc(P.dsem[sig[1]][sig[2]], 16)
            return f

        with nc.Block() as block:
            block.tensor(replay("pe"))
            block.scalar(replay("act"))
            block.vector(replay("dve"))
            block.gpsimd(replay("pool"))
            block.sync(replay("sp"))
        self.es.close()

    def next_psum(self):
        i = self.ps_pool[self.psi % len(self.ps_pool)]
        self.psi += 1
        return self.psum[i], self.psb[i]


D = 2048
SEQ = 8192
NCORE = 8
TPC = SEQ // NCORE
NT = TPC // 128
KC_D = D // 128
IN_COLS = 9216
DFF = 7168
NEXP = 8
EPS = 1e-6
DBG = {}
POOLC = "dve"


def bc_row(t, off, n):
    return bass.AP(t, off, [[0, 128], [1, n]])


class Ctx:
    def __init__(self, P, ident_d):
        self.P = P
        self.idf = P.alloc_f32(128)
        self.b_idf = P.buf("idf")
        self.idb = P.alloc_bf16(128)
        self.b_idb = P.buf("idb")
        self.onesb = P.alloc_bf16(128)
        self.b_ones = P.buf("ones")
        self.eps = P.alloc_f32(2)
        self.b_eps = P.buf("eps")
        P.dma("sp", lambda e: e.dma_start(out=self.idf, in_=ident_d.ap()), writes=[self.b_idf])
        P.op("dve", lambda e: e.tensor_copy(out=self.idb, in_=self.idf), reads=[self.b_idf], writes=[self.b_idb])
        P.op("dve", lambda e: e.memset(self.onesb, 1.0), writes=[self.b_ones])
        P.op("dve", lambda e: e.memset(self.eps, EPS), writes=[self.b_eps])
        self.njunk = 2
        self.junks = [P.alloc_bf16(2048) for _ in range(self.njunk)]
        self.b_junks = [P.buf(f"junk{i}") for i in range(self.njunk)]
        self.ji = 0
        self.nss = 8
        self.ss = [P.alloc_f32(4) for _ in range(self.nss)]
        self.b_ss = [P.buf(f"ss{i}") for i in range(self.nss)]
        self.ssi = 0
        self.evi = 0

    def next_junk(self):
        i = self.ji % self.njunk
        self.ji += 1
        return self.junks[i], self.b_junks[i]

    def next_ss(self):
        i = self.ssi % self.nss
        self.ssi += 1
        return self.ss[i], self.b_ss[i]

    def evac(self, out, in_, reads, writes):
        P = self.P
        self.evi += 1
        if self.evi % 2:
            return P.op("act", lambda e: e.activation(out=out, in_=in_, func=AF.Copy), reads=reads, writes=writes)
        return P.op("dve", lambda e: e.tensor_copy(out=out, in_=in_), reads=reads, writes=writes)


def emit_rstd(P, C, ss, b_ss, n):
    P.op("act", lambda e: e.activation(out=ss[:, 1:2], in_=ss[:, 0:1], func=AF.Sqrt, scale=1.0 / n, bias=C.eps[:, 0:1]),
         reads=[b_ss, C.b_eps], writes=[b_ss])
    P.op("dve", lambda e: e.reciprocal(out=ss[:, 1:2], in_=ss[:, 1:2]), reads=[b_ss], writes=[b_ss])


def emit_rmsnorm(P, C, src, rd, gain, b_gain, out, b_out, n):
    ss, b_ss = C.next_ss()
    jk, b_jk = C.next_junk()
    P.op("act", lambda e: e.activation(out=jk[:, 0:n], in_=src, func=AF.Square, accum_out=ss[:, 0:1]),
         reads=rd, writes=[b_ss, b_jk])
    emit_rstd(P, C, ss, b_ss, n)
    P.op("dve", lambda e: e.scalar_tensor_tensor(out=out, in0=src, scalar=ss[:, 1:2], in1=gain,
                                                  op0=ALU.mult, op1=ALU.mult),
         reads=rd + [b_ss, b_gain], writes=[b_out])


def emit_transposes(P, C, src, b_src, widths, dst_fn, b_dst, f32=False):
    per_bank = 4 if f32 else 8
    ident = C.idf if f32 else C.idb
    b_ident = C.b_idf if f32 else C.b_idb
    j = 0
    c0 = 0
    nb = len(widths)
    while j < nb:
        grp = list(range(j, min(nb, j + per_bank)))
        ps, pb = P.next_psum()
        pv = ps if f32 else ps.bitcast(BF16)
        cc = c0
        for gi, jj in enumerate(grp):
            w = widths[jj]
            P.op("pe", lambda e, gi=gi, w=w, cc=cc, pv=pv: e.transpose(
                out=pv[0:w, gi * 128:(gi + 1) * 128], in_=src[:, cc:cc + w], identity=ident),
                reads=[b_src, b_ident], writes=[pb] if gi == 0 else [], signal=(gi == len(grp) - 1))
            cc += w
        for gi, jj in enumerate(grp):
            w = widths[jj]
            C.evac(dst_fn(jj), pv[0:w, gi * 128:(gi + 1) * 128], [pb], [b_dst])
        c0 = cc
        j += per_bank


def norm_transpose(P, C, NTl, row_src, gain, b_gain, hT3, b_hT, Dm=D, hook=None):
    KC = Dm // 128
    hb = [P.alloc_bf16(Dm) for _ in range(2)]
    b_h = [P.buf("hb0"), P.buf("hb1")]
    for t in range(NTl):
        s = t % 2
        src, rd = row_src(t)
        emit_rmsnorm(P, C, src, rd, gain, b_gain, hb[s], b_h[s], Dm)
        if hook is not None:
            hook(t, src, rd)
        for half in range(KC // 8):
            ps, pb = P.next_psum()
            pv = ps.bitcast(BF16)
            for j in range(8):
                k = half * 8 + j
                P.op("pe", lambda e, s=s, k=k, j=j, pv=pv: e.transpose(
                    out=pv[:, j * 128:(j + 1) * 128], in_=hb[s][:, k * 128:(k + 1) * 128], identity=C.idb),
                    reads=[b_h[s], C.b_idb], writes=[pb] if j == 0 else [], signal=(j == 7))
            C.evac(hT3[:, half * 8:(half + 1) * 8, t * 128:(t + 1) * 128],
                   pv.rearrange("p (k t) -> p k t", k=8), [pb], [b_hT[t]])


def linear_tm(P, C, lhsT3, b_lhs, kparts, NTl, W, w_off, ldw, col_blocks, width, epilogue, nslab=2, pair_off=None):
    KC = len(kparts)
    np_ = 2 if pair_off is not None else 1
    slabs = [[P.alloc_bf16(KC * width) for _ in range(np_)] for _ in range(nslab)]
    b_sl = [[P.buf("slab") for _ in range(np_)] for _ in range(nslab)]
    full = all(k == 128 for k in kparts)
    for bi, c0 in enumerate(col_blocks):
        s = bi % nslab
        for pi in range(np_):
            sl3 = slabs[s][pi].rearrange("p (k n) -> p k n", k=KC)
            coff = w_off + c0 + (pair_off if pi else 0)
            if full:
                src = bass.AP(W, coff, [[ldw, 128], [128 * ldw, KC], [1, width]])
                P.dma("pool", lambda e, sl3=sl3, src=src: e.dma_start(out=sl3, in_=src), writes=[b_sl[s][pi]])
            else:
                nf = sum(1 for k in kparts if k == 128)
                src = bass.AP(W, coff, [[ldw, 128], [128 * ldw, nf], [1, width]])
                P.dma("pool", lambda e, sl3=sl3, src=src, nf=nf: e.dma_start(out=sl3[:, 0:nf, :], in_=src), writes=[b_sl[s][pi]])
                kp = kparts[-1]
                src2 = bass.AP(W, coff + nf * 128 * ldw, [[ldw, kp], [1, width]])
                P.dma("pool", lambda e, sl3=sl3, src2=src2, nf=nf, kp=kp: e.dma_start(out=sl3[0:kp, nf, :], in_=src2),
                      writes=[b_sl[s][pi]])
        for t in range(NTl):
            pss = []
            for pi in range(np_):
                sl3 = slabs[s][pi].rearrange("p (k n) -> p k n", k=KC)
                ps, pb = P.next_psum()
                for k in range(KC):
                    kp = kparts[k]
                    P.op("pe", lambda e, ps=ps, t=t, k=k, kp=kp, sl3=sl3: e.matmul(
                        ps[:, 0:width], lhsT3[0:kp, k, t * 128:(t + 1) * 128], sl3[0:kp, k, :],
                        start=(k == 0), stop=(k == KC - 1)),
                        reads=[b_lhs[t], b_sl[s][pi]], writes=[pb] if k == 0 else [], signal=(k == KC - 1))
                pss += [ps, pb]
            epilogue(t, bi, *pss)


class OutStage:
    def __init__(self, P, w, n=3, dt=F32):
        self.t = [P.alloc_f32(w) if dt == F32 else P.alloc_bf16(w) for _ in range(n)]
        self.b = [P.buf("ost") for _ in range(n)]
        self.i = 0

    def next(self):
        i = self.i % len(self.t)
        self.i += 1
        return self.t[i], self.b[i]


def phase_inproj(P, C, x_d, g_d, g_off, w_d, w_off, proj_d):
    m = P.mark()
    gbc = P.alloc_f32(D)
    b_g = P.buf("gbc")
    P.dma("sp", lambda e: e.dma_start(out=gbc, in_=bc_row(g_d, g_off, D)), writes=[b_g])
    hT = P.alloc_bf16(KC_D * TPC)
    hT3 = hT.rearrange("p (k t) -> p k t", k=KC_D)
    b_hT = [P.buf(f"hT{t}") for t in range(NT)]
    xt = [P.alloc_f32(D) for _ in range(2)]
    b_x = [P.buf("xa"), P.buf("xb")]

    def row_src(t):
        s = t % 2
        P.dma("sp", lambda e: e.dma_start(out=xt[s], in_=x_d.ap()[t * 128:(t + 1) * 128, :]), writes=[b_x[s]])
        return xt[s], [b_x[s]]

    norm_transpose(P, C, NT, row_src, gbc, b_g, hT3, b_hT)
    ost = OutStage(P, 512, 4, BF16)

    def epi(t, bi, ps, pb):
        o, bo = ost.next()
        C.evac(o, ps[:, :], [pb], [bo])
        P.dma("sp", lambda e: e.dma_start(out=proj_d.ap()[t * 128:(t + 1) * 128, bi * 512:(bi + 1) * 512], in_=o), reads=[bo])

    linear_tm(P, C, hT3, b_hT, [128] * KC_D, NT, w_d, w_off, IN_COLS, [i * 512 for i in range(IN_COLS // 512)], 512, epi)
    P.release(m)


def build_inproj():
    nc = bass.Bass("TRN2", target_bir_lowering=False)
    x_d = nc.dram_tensor("x", [TPC, D], F32, kind="ExternalInput")
    g_d = nc.dram_tensor("g", [1, D], F32, kind="ExternalInput")
    w_d = nc.dram_tensor("w", [D, IN_COLS], F32, kind="ExternalInput")
    ident = nc.dram_tensor("ident", [128, 128], F32, kind="ExternalInput")
    proj_d = nc.dram_tensor("proj", [TPC, IN_COLS], BF16, kind="ExternalOutput")
    P = Prog(nc)
    C = Ctx(P, ident)
    phase_inproj(P, C, x_d, g_d, 0, w_d, 0, proj_d)
    P.finish()
    return nc


def phase_ffn(P, C, xacc, b_xacc, g_d, g_off, wgu_list, wd_list, router=None, route_out=None):
    m = P.mark()
    nexp = len(wgu_list)
    gbc = P.alloc_f32(D)
    b_g = P.buf("gbc")
    P.dma("sp", lambda e: e.dma_start(out=gbc, in_=bc_row(g_d, g_off, D)), writes=[b_g])
    hT = P.alloc_bf16(KC_D * TPC)
    hT3 = hT.rearrange("p (k t) -> p k t", k=KC_D)
    b_hT = [P.buf(f"hT{t}") for t in range(NT)]
    wts = P.alloc_f32(NT * NEXP)
    b_wts = [P.buf(f"wts{t}") for t in range(NT)]
    m2 = P.mark()
    hook = None
    post = None
    if router is not None:
        rT_d, r_off, rb_d, rb_off = router
        HE = NEXP // 2
        gr = P.alloc_f32(HE * D)
        b_gr = P.buf("gr")
        rb = P.alloc_f32(NEXP)
        b_rb = P.buf("rb")
        P.dma("sp", lambda e: e.dma_start(out=rb, in_=bc_row(rb_d, rb_off, NEXP)), writes=[b_rb])
        raw = P.alloc_f32(NT * NEXP)
        rst = P.alloc_f32(NT)
        b_raw = [P.buf(f"raw{t}") for t in range(NT)]
        lg = [P.alloc_f32(64) for _ in range(2)]
        b_lg = [P.buf("lg0"), P.buf("lg1")]
        jf = P.alloc_f32(D)
        b_jf = P.buf("jf")

        def load_gr(half):
            P.dma("sp", lambda e: e.dma_start(out=gr, in_=bc_row(rT_d, r_off + half * HE * D, HE * D)), writes=[b_gr])
            for ex in range(HE):
                P.op(POOLC, lambda e, ex=ex: e.tensor_tensor(out=gr[:, ex * D:(ex + 1) * D], in0=gr[:, ex * D:(ex + 1) * D],
                                                             in1=gbc, op=ALU.mult), reads=[b_gr, b_g], writes=[b_gr])

        def raw_logits(t, src, rd, half):
            for ex in range(HE):
                col = t * NEXP + half * HE + ex
                P.op("dve", lambda e, ex=ex, col=col: e.scalar_tensor_tensor(
                    out=jf, in0=src, scalar=1.0, in1=gr[:, ex * D:(ex + 1) * D], op0=ALU.mult, op1=ALU.mult,
                    accum_out=raw[:, col:col + 1]), reads=rd + [b_gr], writes=[b_raw[t], b_jf])

        load_gr(0)

        def hook(t, src, rd):
            ss, b_ss = C.ss[(C.ssi - 1) % C.nss], C.b_ss[(C.ssi - 1) % C.nss]
            P.op("dve", lambda e: e.tensor_copy(out=rst[:, t:t + 1], in_=ss[:, 1:2]), reads=[b_ss], writes=[b_raw[t]])
            raw_logits(t, src, rd, 0)

        def post():
            load_gr(1)
            for t in range(NT):
                raw_logits(t, xacc[t], [b_xacc[t]], 1)
                L, bL = lg[t % 2], b_lg[t % 2]
                P.op("dve", lambda e, L=L, t=t: e.scalar_tensor_tensor(out=L[:, 8:16], in0=raw[:, t * NEXP:(t + 1) * NEXP],
                                                                      scalar=rst[:, t:t + 1], in1=rb, op0=ALU.mult, op1=ALU.add),
                     reads=[b_raw[t], b_rb], writes=[bL])
                lgt = L[:, 8:16]

                def lo(fn, eng="dve", L=L, bL=bL):
                    P.op(eng, fn, reads=[bL], writes=[bL])
                lo(lambda e, L=L, lgt=lgt: e.reduce_max(out=L[:, 16:17], in_=lgt, axis=AX.X))
                lo(lambda e, L=L, lgt=lgt: e.tensor_scalar(out=L[:, 24:32], in0=lgt, scalar1=L[:, 16:17], scalar2=None, op0=ALU.is_equal))
                lo(lambda e, L=L, lgt=lgt: e.scalar_tensor_tensor(out=L[:, 32:40], in0=L[:, 24:32], scalar=-1e30, in1=lgt,
                                                                 op0=ALU.mult, op1=ALU.add))
                lo(lambda e, L=L: e.reduce_max(out=L[:, 17:18], in_=L[:, 32:40], axis=AX.X))
                lo(lambda e, L=L: e.tensor_scalar(out=L[:, 40:48], in0=L[:, 32:40], scalar1=L[:, 17:18], scalar2=None, op0=ALU.is_equal))
                lo(lambda e, L=L: e.tensor_tensor(out=L[:, 18:19], in0=L[:, 16:17], in1=L[:, 17:18], op=ALU.subtract))
                lo(lambda e, L=L: e.activation(out=L[:, 19:20], in_=L[:, 18:19], func=AF.Sigmoid), "act")
                lo(lambda e, L=L: e.activation(out=L[:, 20:21], in_=L[:, 18:19], func=AF.Sigmoid, scale=-1.0), "act")
                wt = wts[:, t * NEXP:(t + 1) * NEXP]
                P.op("dve", lambda e, L=L, wt=wt: e.tensor_scalar(out=wt, in0=L[:, 24:32], scalar1=L[:, 19:20], scalar2=None,
                                                                  op0=ALU.mult), reads=[bL], writes=[b_wts[t]])
                P.op("dve", lambda e, L=L, wt=wt: e.scalar_tensor_tensor(out=wt, in0=L[:, 40:48], scalar=L[:, 20:21], in1=wt,
                                                                         op0=ALU.mult, op1=ALU.add), reads=[bL, b_wts[t]], writes=[b_wts[t]])

    def row_src(t):
        return xacc[t], [b_xacc[t]]

    norm_transpose(P, C, NT, row_src, gbc, b_g, hT3, b_hT, hook=hook)
    if post is not None:
        post()
    P.release(m2)
    if route_out is not None:
        hT_d, wts_d = route_out
        for half in range(2):
            P.dma("sp", lambda e, half=half: e.dma_start(
                out=bass.AP(hT_d, half * 8 * 128 * TPC, [[TPC, 128], [128 * TPC, 8], [1, TPC]]),
                in_=hT3[:, half * 8:(half + 1) * 8, :]), reads=b_hT)
        P.dma("sp", lambda e: e.dma_start(out=bass.AP(wts_d, 0, [[NEXP, 128], [128 * NEXP, NT], [1, NEXP]]),
                                          in_=wts.rearrange("p (t e) -> p t e", e=NEXP)), reads=b_wts)
        P.release(m)
        return
    wcol_fn = None
    if router is not None:
        wcol_fn = lambda t, ex: (wts[:, t * NEXP + ex:t * NEXP + ex + 1], b_wts[t])
    ffn_body(P, C, hT3, b_hT, xacc, b_xacc, wgu_list, wd_list, wcol_fn)
    P.release(m)


def ffn_body(P, C, hT3, b_hT, xacc, b_xacc, wgu_list, wd_list, wcol_fn=None, init_zero=False):
    mm = P.mark()
    nexp = len(wgu_list)
    FB = 256
    NFB = DBG.get('nfb', DFF // FB)
    wg = [P.alloc_bf16(KC_D * FB) for _ in range(2)]
    wu = [P.alloc_bf16(KC_D * FB) for _ in range(2)]
    wd = [P.alloc_bf16(2 * D) for _ in range(2)]
    b_wg = [P.buf("wg0"), P.buf("wg1")]
    b_wu = [P.buf("wu0"), P.buf("wu1")]
    b_wd = [P.buf("wd0"), P.buf("wd1")]
    actT = [P.alloc_bf16(2 * TPC) for _ in range(2)]
    b_act = [P.buf("act0"), P.buf("act1")]
    sg = [P.alloc_f32(512) for _ in range(2)]
    b_sg = [P.buf("sg0"), P.buf("sg1")]
    it = 0
    for ex in range(nexp):
        wgu_d, wgu_off = wgu_list[ex]
        wd_d, wd_off = wd_list[ex]
        for fb in range(NFB):
            s = it % 2
            first = (it == 0)
            it += 1
            wg3 = wg[s].rearrange("p (k n) -> p k n", k=KC_D)
            wu3 = wu[s].rearrange("p (k n) -> p k n", k=KC_D)
            wd3 = wd[s].rearrange("p (c n) -> p c n", c=2)
            a3 = actT[s].rearrange("p (c t) -> p c t", c=2)
            P.dma("pool", lambda e, wg3=wg3, o=wgu_off + fb * FB, wgu_d=wgu_d: e.dma_start(
                out=wg3, in_=bass.AP(wgu_d, o, [[2 * DFF, 128], [128 * 2 * DFF, KC_D], [1, FB]])), writes=[b_wg[s]])
            P.dma("pool", lambda e, wu3=wu3, o=wgu_off + DFF + fb * FB, wgu_d=wgu_d: e.dma_start(
                out=wu3, in_=bass.AP(wgu_d, o, [[2 * DFF, 128], [128 * 2 * DFF, KC_D], [1, FB]])), writes=[b_wu[s]])
            P.dma("pool", lambda e, wd3=wd3, o=wd_off + fb * FB * D, wd_d=wd_d: e.dma_start(
                out=wd3, in_=bass.AP(wd_d, o, [[D, 128], [128 * D, 2], [1, D]])), writes=[b_wd[s]])
            for tb in range(TPC // 512):
                for c in range(2):
                    psg, pbg = P.next_psum()
                    for k in range(KC_D):
                        P.op("pe", lambda e, psg=psg, k=k, c=c, tb=tb, wg3=wg3: e.matmul(
                            psg[:, :], wg3[:, k, c * 128:(c + 1) * 128], hT3[:, k, tb * 512:(tb + 1) * 512],
                            start=(k == 0), stop=(k == KC_D - 1)),
                            reads=[b_wg[s]] + b_hT[tb * 4:(tb + 1) * 4], writes=[pbg], signal=(k == KC_D - 1))
                    psu, pbu = P.next_psum()
                    for k in range(KC_D):
                        P.op("pe", lambda e, psu=psu, k=k, c=c, tb=tb, wu3=wu3: e.matmul(
                            psu[:, :], wu3[:, k, c * 128:(c + 1) * 128], hT3[:, k, tb * 512:(tb + 1) * 512],
                            start=(k == 0), stop=(k == KC_D - 1)),
                            reads=[b_wu[s]] + b_hT[tb * 4:(tb + 1) * 4], writes=[pbu], signal=(k == KC_D - 1))
                    si = (tb * 2 + c) % 2
                    P.op("act", lambda e, psg=psg, si=si: e.activation(out=sg[si], in_=psg[:, :], func=AF.Sigmoid),
                         reads=[pbg], writes=[b_sg[si]])
                    P.op("dve", lambda e, psg=psg, si=si: e.tensor_tensor(out=sg[si], in0=psg[:, :], in1=sg[si], op=ALU.mult),
                         reads=[pbg, b_sg[si]], writes=[b_sg[si]])
                    P.op("dve", lambda e, psu=psu, si=si, a3=a3, c=c, tb=tb: e.tensor_tensor(
                        out=a3[:, c, tb * 512:(tb + 1) * 512], in0=psu[:, :], in1=sg[si], op=ALU.mult),
                        reads=[pbu, b_sg[si]], writes=[b_act[s]])
            for t in range(NT):
                for nb in range(D // 512):
                    ps, pb = P.next_psum()
                    for c in range(2):
                        P.op("pe", lambda e, ps=ps, c=c, t=t, nb=nb, a3=a3, wd3=wd3: e.matmul(
                            ps[:, :], a3[:, c, t * 128:(t + 1) * 128], wd3[:, c, nb * 512:(nb + 1) * 512],
                            start=(c == 0), stop=(c == 1)),
                            reads=[b_act[s], b_wd[s]], writes=[pb], signal=(c == 1))
                    xa = xacc[t][:, nb * 512:(nb + 1) * 512]
                    if wcol_fn is None:
                        P.op("dve", lambda e, ps=ps, xa=xa: e.tensor_tensor(out=xa, in0=ps[:, :], in1=xa, op=ALU.add),
                             reads=[pb, b_xacc[t]], writes=[b_xacc[t]])
                    else:
                        wcol, b_w = wcol_fn(t, ex)
                        if init_zero and first:
                            P.op("dve", lambda e, ps=ps, xa=xa, wcol=wcol: e.tensor_scalar(
                                out=xa, in0=ps[:, :], scalar1=wcol, scalar2=None, op0=ALU.mult),
                                reads=[pb, b_w], writes=[b_xacc[t]])
                        else:
                            P.op("dve", lambda e, ps=ps, xa=xa, wcol=wcol: e.scalar_tensor_tensor(
                                out=xa, in0=ps[:, :], scalar=wcol, in1=xa, op0=ALU.mult, op1=ALU.add),
                                reads=[pb, b_xacc[t], b_w], writes=[b_xacc[t]])
    P.release(mm)


def build_ffn(moe):
    nc = bass.Bass("TRN2", target_bir_lowering=False)
    x_d = nc.dram_tensor("x", [TPC, D], F32, kind="ExternalInput")
    g_d = nc.dram_tensor("g", [1, D], F32, kind="ExternalInput")
    ne = NEXP if moe else 1
    wgu_d = nc.dram_tensor("wgu", [ne, D, 2 * DFF], F32, kind="ExternalInput")
    wd_d = nc.dram_tensor("wd", [ne, DFF, D], F32, kind="ExternalInput")
    ident = nc.dram_tensor("ident", [128, 128], F32, kind="ExternalInput")
    router = None
    if moe:
        rT_d = nc.dram_tensor("rT", [NEXP, D], F32, kind="ExternalInput")
        rb_d = nc.dram_tensor("rb", [1, NEXP], F32, kind="ExternalInput")
        router = (rT_d, 0, rb_d, 0)
    y_d = nc.dram_tensor("y", [TPC, D], F32, kind="ExternalOutput")
    P = Prog(nc)
    C = Ctx(P, ident)
    xacc = [P.alloc_f32(D) for _ in range(NT)]
    b_xacc = [P.buf(f"xacc{t}") for t in range(NT)]
    for t in range(NT):
        P.dma("sp", lambda e, t=t: e.dma_start(out=xacc[t], in_=x_d.ap()[t * 128:(t + 1) * 128, :]), writes=[b_xacc[t]])
    phase_ffn(P, C, xacc, b_xacc, g_d, 0, [(wgu_d, ex * D * 2 * DFF) for ex in range(ne)],
              [(wd_d, ex * DFF * D) for ex in range(ne)], router)
    for t in range(NT):
        P.dma("sp", lambda e, t=t: e.dma_start(out=y_d.ap()[t * 128:(t + 1) * 128, :], in_=xacc[t]), reads=[b_xacc[t]])
    P.finish()
    return nc


PI = 3.14159265358979
TWO_PI = 2.0 * PI


def emit_sincos(P, C, x, b_x, n, out_s, out_c, b_out, tmp, ti, b_tmp):
    y = tmp[:, 0:n]
    kf = tmp[:, n:2 * n]
    for shift, out in ((0.0, out_s), (PI / 2, out_c)):
        P.op("dve", lambda e, shift=shift: e.tensor_scalar(out=y, in0=x, scalar1=shift, scalar2=None, op0=ALU.add),
             reads=[b_x], writes=[b_tmp])
        P.op("dve", lambda e: e.tensor_scalar(out=ti, in0=y, scalar1=1.0 / TWO_PI, scalar2=None, op0=ALU.mult),
             reads=[b_tmp], writes=[b_tmp])
        P.op("dve", lambda e: e.tensor_copy(out=kf, in_=ti), reads=[b_tmp], writes=[b_tmp])
        P.op("dve", lambda e: e.scalar_tensor_tensor(out=y, in0=kf, scalar=-TWO_PI, in1=y, op0=ALU.mult, op1=ALU.add),
             reads=[b_tmp], writes=[b_tmp])
        P.op("dve", lambda e: e.tensor_scalar(out=kf, in0=y, scalar1=PI, scalar2=None, op0=ALU.is_gt),
             reads=[b_tmp], writes=[b_tmp])
        P.op("dve", lambda e: e.scalar_tensor_tensor(out=y, in0=kf, scalar=-TWO_PI, in1=y, op0=ALU.mult, op1=ALU.add),
             reads=[b_tmp], writes=[b_tmp])
        P.op("dve", lambda e: e.tensor_scalar(out=kf, in0=y, scalar1=-PI, scalar2=None, op0=ALU.is_lt),
             reads=[b_tmp], writes=[b_tmp])
        P.op("dve", lambda e: e.scalar_tensor_tensor(out=y, in0=kf, scalar=TWO_PI, in1=y, op0=ALU.mult, op1=ALU.add),
             reads=[b_tmp], writes=[b_tmp])
        P.op("dve", lambda e: e.tensor_scalar(out=y, in0=y, scalar1=-3.1415925, scalar2=3.1415925, op0=ALU.max, op1=ALU.min),
             reads=[b_tmp], writes=[b_tmp])
        P.op("act", lambda e, out=out: e.activation(out=out, in_=y, func=AF.Sin), reads=[b_tmp], writes=[b_out])


S5C = 512


def phase_s5(P, C, u_d, Bre_d, Bim_d, Cre_d, Cim_d, par_d, ramp_d, ysT_d):
    m = P.mark()
    nch = SEQ // S5C
    Bre = P.alloc_f32(512); Bim = P.alloc_f32(512); Cre = P.alloc_f32(512); Cimn = P.alloc_f32(512)
    b_par = P.buf("s5par")
    par = P.alloc_f32(64)
    for dst, src in ((Bre, Bre_d), (Bim, Bim_d), (Cre, Cre_d), (Cimn, Cim_d)):
        P.dma("sp", lambda e, dst=dst, src=src: e.dma_start(out=dst, in_=src.ap()), writes=[b_par])
    P.dma("sp", lambda e: e.dma_start(out=par[:, 0:16], in_=par_d.ap()), writes=[b_par])
    P.op("dve", lambda e: e.tensor_scalar(out=Cimn, in0=Cimn, scalar1=-1.0, scalar2=None, op0=ALU.mult),
         reads=[b_par], writes=[b_par])
    Breb = P.alloc_bf16(512); Bimb = P.alloc_bf16(512); Creb = P.alloc_bf16(512); Cimnb = P.alloc_bf16(512)
    for dst, src in ((Breb, Bre), (Bimb, Bim), (Creb, Cre), (Cimnb, Cimn)):
        P.op("dve", lambda e, dst=dst, src=src: e.tensor_copy(out=dst, in_=src), reads=[b_par], writes=[b_par])
    are, aim, ldt, dcol = par[:, 0:4], par[:, 4:8], par[:, 8:12], par[:, 12:13]
    dt, rr, th, cth, sth = par[:, 16:20], par[:, 20:24], par[:, 24:28], par[:, 28:32], par[:, 32:36]
    nre, nim, den, fre, fim = par[:, 36:40], par[:, 40:44], par[:, 44:48], par[:, 48:52], par[:, 52:56]
    t1, t2 = par[:, 56:60], par[:, 60:64]
    sc_tmp = P.alloc_f32(2 * 512)
    sc_ti = P.alloc(512).bitcast(I32)
    b_sc = P.buf("sctmp")

    def pv(fn, eng="dve"):
        P.op(eng, fn, reads=[b_par], writes=[b_par])

    pv(lambda e: e.activation(out=dt, in_=ldt, func=AF.Exp), "act")
    pv(lambda e: e.tensor_tensor(out=t1, in0=dt, in1=are, op=ALU.mult))
    pv(lambda e: e.activation(out=rr, in_=t1, func=AF.Exp), "act")
    pv(lambda e: e.tensor_tensor(out=th, in0=dt, in1=aim, op=ALU.mult))
    emit_sincos(P, C, th, b_par, 4, sth, cth, b_par, sc_tmp[:, 0:8], sc_ti[:, 0:4], b_sc)
    pv(lambda e: e.tensor_tensor(out=nre, in0=rr, in1=cth, op=ALU.mult))
    pv(lambda e: e.tensor_scalar(out=nre, in0=nre, scalar1=-1.0, scalar2=None, op0=ALU.add))
    pv(lambda e: e.tensor_tensor(out=nim, in0=rr, in1=sth, op=ALU.mult))
    pv(lambda e: e.tensor_tensor(out=t1, in0=are, in1=are, op=ALU.mult))
    pv(lambda e: e.tensor_tensor(out=t2, in0=aim, in1=aim, op=ALU.mult))
    pv(lambda e: e.tensor_tensor(out=den, in0=t1, in1=t2, op=ALU.add))
    pv(lambda e: e.reciprocal(out=den, in_=den))
    pv(lambda e: e.tensor_tensor(out=t1, in0=nre, in1=are, op=ALU.mult))
    pv(lambda e: e.tensor_tensor(out=t2, in0=nim, in1=aim, op=ALU.mult))
    pv(lambda e: e.tensor_tensor(out=fre, in0=t1, in1=t2, op=ALU.add))
    pv(lambda e: e.tensor_tensor(out=fre, in0=fre, in1=den, op=ALU.mult))
    pv(lambda e: e.tensor_tensor(out=t1, in0=nim, in1=are, op=ALU.mult))
    pv(lambda e: e.tensor_tensor(out=t2, in0=nre, in1=aim, op=ALU.mult))
    pv(lambda e: e.tensor_tensor(out=fim, in0=t1, in1=t2, op=ALU.subtract))
    pv(lambda e: e.tensor_tensor(out=fim, in0=fim, in1=den, op=ALU.mult))
    ramp = P.alloc_f32(512)
    b_ramp = P.buf("ramp")
    P.dma("sp", lambda e: e.dma_start(out=ramp, in_=bc_row(ramp_d, 0, 512)), writes=[b_ramp])
    ang = P.alloc_f32(512)
    b_ang = P.buf("ang")
    cosT = [P.alloc_f32(512) for _ in range(4)]
    sinT = [P.alloc_f32(512) for _ in range(4)]
    Gre = [P.alloc_f32(512) for _ in range(4)]
    Gim = [P.alloc_f32(512) for _ in range(4)]
    rbc = [P.alloc_f32(512) for _ in range(4)]
    b_tab = P.buf("tab")
    tq = P.alloc_f32(512)
    for j in range(4):
        P.op("dve", lambda e, j=j: e.tensor_scalar(out=ang, in0=ramp, scalar1=th[:, j:j + 1], scalar2=None, op0=ALU.mult),
             reads=[b_ramp, b_par, b_sc], writes=[b_ang])
        emit_sincos(P, C, ang, b_ang, 512, sinT[j], cosT[j], b_tab, sc_tmp, sc_ti, b_sc)
        P.op("dve", lambda e, j=j: e.tensor_scalar(out=Gre[j], in0=cosT[j], scalar1=fre[:, j:j + 1], scalar2=None, op0=ALU.mult),
             reads=[b_tab, b_par], writes=[b_tab])
        P.op("dve", lambda e, j=j: e.scalar_tensor_tensor(out=Gre[j], in0=sinT[j], scalar=fim[:, j:j + 1], in1=Gre[j],
                                                           op0=ALU.mult, op1=ALU.add), reads=[b_tab, b_par], writes=[b_tab])
        P.op("dve", lambda e, j=j: e.tensor_scalar(out=tq, in0=sinT[j], scalar1=fre[:, j:j + 1], scalar2=None, op0=ALU.mult),
             reads=[b_tab, b_par], writes=[b_tab])
        P.op("dve", lambda e, j=j: e.scalar_tensor_tensor(out=Gim[j], in0=cosT[j], scalar=fim[:, j:j + 1], in1=tq,
                                                           op0=ALU.mult, op1=ALU.subtract), reads=[b_tab, b_par], writes=[b_tab])
        P.op("dve", lambda e, j=j: e.memset(rbc[j], 1.0), writes=[b_tab])
        P.op("dve", lambda e, j=j: e.tensor_scalar(out=rbc[j], in0=rbc[j], scalar1=rr[:, j:j + 1], scalar2=None, op0=ALU.mult),
             reads=[b_tab, b_par], writes=[b_tab])
    uTb = P.alloc_bf16(SEQ)
    b_uT = [P.buf(f"uT{c}") for c in range(SEQ // S5C)]
    ul = [P.alloc_bf16(8 * 128) for _ in range(2)]
    b_ul = [P.buf("ul0"), P.buf("ul1")]
    for g8 in range(SEQ // 1024):
        s = g8 % 2
        P.dma("sp", lambda e, s=s, g8=g8: e.dma_start(
            out=ul[s].rearrange("p (t c) -> p t c", t=8),
            in_=bass.AP(u_d, g8 * 1024 * 128, [[128, 128], [128 * 128, 8], [1, 128]])), writes=[b_ul[s]])
        ps, pb = P.next_psum()
        pv_ = ps.bitcast(BF16)
        for q in range(8):
            P.op("pe", lambda e, pv_=pv_, q=q, s=s: e.transpose(
                out=pv_[:, q * 128:(q + 1) * 128], in_=ul[s][:, q * 128:(q + 1) * 128], identity=C.idb),
                reads=[b_ul[s], C.b_idb], writes=[pb], signal=(q == 7))
        for half in range(2):
            c0 = g8 * 1024 + half * 512
            C.evac(uTb[:, c0:c0 + 512], pv_[:, half * 512:(half + 1) * 512], [pb], [b_uT[c0 // S5C]])
    P.ps_pool = [0, 1, 2, 3, 4, 5]
    if DBG.get("s5_stop", 9) <= 3:
        nch = 0
    nch = min(nch, DBG.get("s5_nch", nch))
    sre = [P.alloc_f32(S5C) for _ in range(4)]
    sim = [P.alloc_f32(S5C) for _ in range(4)]
    b_st = [P.buf(f"st{j}") for j in range(4)]
    bur = [P.alloc_f32(S5C) for _ in range(2)]
    bui = [P.alloc_f32(S5C) for _ in range(2)]
    b_bu = [P.buf("bu0"), P.buf("bu1")]
    ta = [P.alloc_f32(S5C) for _ in range(2)]
    tb = [P.alloc_f32(S5C) for _ in range(2)]
    b_t = [P.buf("t0"), P.buf("t1")]
    vr = [P.alloc_f32(S5C) for _ in range(2)]
    vi = [P.alloc_f32(S5C) for _ in range(2)]
    b_v = [P.buf("v0"), P.buf("v1")]
    srb = [P.alloc_bf16(S5C) for _ in range(2)]
    sib = [P.alloc_bf16(S5C) for _ in range(2)]
    b_sb = [P.buf("sb0"), P.buf("sb1")]
    yf = P.alloc_f32(S5C); yt = P.alloc_f32(S5C); ysg = P.alloc_f32(S5C)
    b_y = P.buf("yf")
    yo = [P.alloc_bf16(S5C) for _ in range(2)]
    b_yo = [P.buf("yo0"), P.buf("yo1")]
    YB = [6, 7]
    it = 0
    for c in range(nch):
        ucb = uTb[:, c * S5C:(c + 1) * S5C]
        ucs = ucb
        psy, pby = P.psum[YB[c % 2]], P.psb[YB[c % 2]]
        for j in range(4):
            s = it % 2
            it += 1
            psr, pbr = P.next_psum()
            psi_, pbi = P.next_psum()
            P.op("pe", lambda e, psr=psr, j=j, ucb=ucb: e.matmul(psr[:, :], Breb[:, j * 128:(j + 1) * 128], ucb, start=True, stop=True),
                 reads=[b_par, b_uT[c]], writes=[pbr])
            P.op("pe", lambda e, psi_=psi_, j=j, ucb=ucb: e.matmul(psi_[:, :], Bimb[:, j * 128:(j + 1) * 128], ucb, start=True, stop=True),
                 reads=[b_par, b_uT[c]], writes=[pbi])
            P.op("dve", lambda e, s=s, j=j, psr=psr: e.tensor_tensor(out=ta[s], in0=psr[:, :], in1=Gre[j], op=ALU.mult),
                 reads=[pbr, b_tab], writes=[b_t[s]])
            P.op("dve", lambda e, s=s, j=j, psi_=psi_: e.tensor_tensor(out=tb[s], in0=psi_[:, :], in1=Gim[j], op=ALU.mult),
                 reads=[pbi, b_tab], writes=[b_t[s]])
            P.op(POOLC, lambda e, s=s: e.tensor_tensor(out=bur[s], in0=ta[s], in1=tb[s], op=ALU.subtract),
                 reads=[b_t[s]], writes=[b_bu[s]])
            P.op("dve", lambda e, s=s, j=j, psi_=psi_: e.tensor_tensor(out=ta[s], in0=psi_[:, :], in1=Gre[j], op=ALU.mult),
                 reads=[pbi, b_tab], writes=[b_t[s]])
            P.op("dve", lambda e, s=s, j=j, psr=psr: e.tensor_tensor(out=tb[s], in0=psr[:, :], in1=Gim[j], op=ALU.mult),
                 reads=[pbr, b_tab], writes=[b_t[s]])
            P.op(POOLC, lambda e, s=s: e.tensor_tensor(out=bui[s], in0=ta[s], in1=tb[s], op=ALU.add),
                 reads=[b_t[s]], writes=[b_bu[s]])
            ir = 0.0 if c == 0 else sre[j][:, S5C - 1:S5C]
            ii = 0.0 if c == 0 else sim[j][:, S5C - 1:S5C]
            P.op("dve", (lambda e, s=s, j=j, ir=ir: e.tensor_copy(out=vr[s], in_=bur[s])) if DBG.get("noscan") else lambda e, s=s, j=j, ir=ir: e.tensor_tensor_scan(out=vr[s], data0=rbc[j], data1=bur[s], initial=ir,
                                                                       op0=ALU.mult, op1=ALU.add),
                 reads=[b_bu[s], b_tab, b_st[j]], writes=[b_v[s]])
            P.op("dve", (lambda e, s=s, j=j, ii=ii: e.tensor_copy(out=vi[s], in_=bui[s])) if DBG.get("noscan") else lambda e, s=s, j=j, ii=ii: e.tensor_tensor_scan(out=vi[s], data0=rbc[j], data1=bui[s], initial=ii,
                                                                       op0=ALU.mult, op1=ALU.add),
                 reads=[b_bu[s], b_tab, b_st[j]], writes=[b_v[s]])
            P.op(POOLC, lambda e, s=s, j=j: e.tensor_tensor(out=sre[j], in0=vr[s], in1=cosT[j], op=ALU.mult),
                 reads=[b_v[s], b_tab], writes=[b_st[j]])
            P.op(POOLC, lambda e, s=s, j=j: e.tensor_tensor(out=ta[s], in0=vi[s], in1=sinT[j], op=ALU.mult),
                 reads=[b_v[s], b_tab], writes=[b_t[s]])
            P.op(POOLC, lambda e, s=s, j=j: e.tensor_tensor(out=sre[j], in0=sre[j], in1=ta[s], op=ALU.subtract),
                 reads=[b_t[s], b_st[j]], writes=[b_st[j]])
            P.op(POOLC, lambda e, s=s, j=j: e.tensor_tensor(out=sim[j], in0=vr[s], in1=sinT[j], op=ALU.mult),
                 reads=[b_v[s], b_tab], writes=[b_st[j]])
            P.op(POOLC, lambda e, s=s, j=j: e.tensor_tensor(out=tb[s], in0=vi[s], in1=cosT[j], op=ALU.mult),
                 reads=[b_v[s], b_tab], writes=[b_t[s]])
            P.op("dve", lambda e, s=s, j=j: e.tensor_tensor(out=sim[j], in0=sim[j], in1=tb[s], op=ALU.add),
                 reads=[b_t[s], b_st[j]], writes=[b_st[j]])
            P.op("act", lambda e, s=s, j=j: e.activation(out=srb[s], in_=sre[j], func=AF.Copy), reads=[b_st[j]], writes=[b_sb[s]])
            P.op("act", lambda e, s=s, j=j: e.activation(out=sib[s], in_=sim[j], func=AF.Copy), reads=[b_st[j]], writes=[b_sb[s]])
            P.op("pe", lambda e, j=j, s=s, psy=psy: e.matmul(psy[:, :], Creb[:, j * 128:(j + 1) * 128], srb[s], start=(j == 0), stop=False),
                 reads=[b_par, b_sb[s]], writes=[pby] if j == 0 else [], signal=False)
            P.op("pe", lambda e, j=j, s=s, psy=psy: e.matmul(psy[:, :], Cimnb[:, j * 128:(j + 1) * 128], sib[s], start=False, stop=(j == 3)),
                 reads=[b_par, b_sb[s]], writes=[pby] if j == 3 else [], signal=True)
        P.op("dve", lambda e, psy=psy, ucs=ucs: e.scalar_tensor_tensor(out=yf, in0=ucs, scalar=dcol, in1=psy[:, :],
                                                                       op0=ALU.mult, op1=ALU.add),
             reads=[pby, b_uT[c], b_par], writes=[b_y])
        P.op(POOLC, lambda e: e.tensor_tensor(out=yt, in0=yf, in1=yf, op=ALU.mult), reads=[b_y], writes=[b_y])
        P.op(POOLC, lambda e: e.tensor_scalar(out=yt, in0=yt, scalar1=0.044715, scalar2=1.0, op0=ALU.mult, op1=ALU.add),
             reads=[b_y], writes=[b_y])
        P.op(POOLC, lambda e: e.tensor_tensor(out=yt, in0=yt, in1=yf, op=ALU.mult), reads=[b_y], writes=[b_y])
        P.op("act", lambda e: e.activation(out=ysg, in_=yt, func=AF.Sigmoid, scale=1.5957691216), reads=[b_y], writes=[b_y])
        so = c % 2
        P.op("dve", lambda e, so=so: e.tensor_tensor(out=yo[so], in0=yf, in1=ysg, op=ALU.mult), reads=[b_y], writes=[b_yo[so]])
        P.dma("sp", lambda e, so=so, c=c: e.dma_start(out=ysT_d.ap()[:, c * S5C:(c + 1) * S5C], in_=yo[so]), reads=[b_yo[so]])
    P.ps_pool = list(range(8))
    P.release(m)


def s5_layouts(l, c, b_re, b_im, c_re, c_im, a_re, a_im, log_dt, dvec):
    Bre = np.zeros((128, 4, 128), np.float32); Bim = np.zeros((128, 4, 128), np.float32)
    Cre = np.zeros((128, 4, 128), np.float32); Cim = np.zeros((128, 4, 128), np.float32)
    par = np.zeros((128, 16), np.float32)
    for j in range(4):
        for s in range(2):
            g = 8 * c + 2 * j + s
            ch = 16 * (2 * j + s)
            Bre[ch:ch + 16, j, 64 * s:64 * s + 64] = b_re[l, g].T
            Bim[ch:ch + 16, j, 64 * s:64 * s + 64] = b_im[l, g].T
            Cre[64 * s:64 * s + 64, j, ch:ch + 16] = c_re[l, g].T
            Cim[64 * s:64 * s + 64, j, ch:ch + 16] = c_im[l, g].T
            par[64 * s:64 * s + 64, j] = a_re[l, g]
            par[64 * s:64 * s + 64, 4 + j] = a_im[l, g]
            par[64 * s:64 * s + 64, 8 + j] = log_dt[l, g]
    par[:, 12] = dvec[l, 8 * c:8 * c + 8].reshape(128)
    return (Bre.reshape(128, 512), Bim.reshape(128, 512), Cre.reshape(128, 512), Cim.reshape(128, 512), par)


def build_s5():
    nc = bass.Bass("TRN2", target_bir_lowering=False)
    u_d = nc.dram_tensor("u", [SEQ, 128], BF16, kind="ExternalInput")
    Bre_d = nc.dram_tensor("Bre", [128, 512], F32, kind="ExternalInput")
    Bim_d = nc.dram_tensor("Bim", [128, 512], F32, kind="ExternalInput")
    Cre_d = nc.dram_tensor("Cre", [128, 512], F32, kind="ExternalInput")
    Cim_d = nc.dram_tensor("Cim", [128, 512], F32, kind="ExternalInput")
    par_d = nc.dram_tensor("par", [128, 16], F32, kind="ExternalInput")
    ramp_d = nc.dram_tensor("ramp", [1, 512], F32, kind="ExternalInput")
    ident = nc.dram_tensor("ident", [128, 128], F32, kind="ExternalInput")
    ys_d = nc.dram_tensor("ysT", [128, SEQ], BF16, kind="ExternalOutput")
    P = Prog(nc)
    C = Ctx(P, ident)
    phase_s5(P, C, u_d, Bre_d, Bim_d, Cre_d, Cim_d, par_d, ramp_d, ys_d)
    P.finish()
    return nc


_IDENT = np.eye(128, dtype=np.float32)
_RAMP = np.arange(1, 513, dtype=np.float32)[None]
_CACHE = {}


def _prog(name, fn, *a):
    key = (name,) + tuple(a)
    if key not in _CACHE:
        _CACHE[key] = fn(*a)
    return _CACHE[key]


def _launch(nc, in_maps):
    res = run_bass_kernel_spmd(nc, in_maps, core_ids=list(range(NCORE)))
    return res.results


def kernel(**inp):
    f32 = np.float32
    x = np.ascontiguousarray(inp["x"][0]).astype(f32, copy=False)
    pos = np.ascontiguousarray(inp["positions"][0]).astype(np.int32, copy=False)
    pos_row = pos.reshape(1, SEQ).view(f32)
    posT_all = np.ascontiguousarray(pos.reshape(SEQ // 128, 128).T).view(f32)
    invf = (10000.0 ** (-np.arange(0, 64, 2, dtype=np.float32) / 64.0)).astype(f32)[None]
    mem = np.ascontiguousarray(inp["mem"][0])
    sl = lambda i: slice(i * TPC, (i + 1) * TPC)
    cc = np.ascontiguousarray
    for l in range(2):
        outs = _launch(_prog("inproj", build_inproj),
                       [{"x": x[sl(i)], "g": inp["norm_mix"][l:l + 1], "w": inp["w_in"][l], "ident": _IDENT} for i in range(NCORE)])
        proj = np.concatenate([o["proj"] for o in outs], 0)
        maps = []
        for c in range(NCORE):
            Bre, Bim, Cre, Cim, par = s5_layouts(l, c, inp["s5_b_re"], inp["s5_b_im"], inp["s5_c_re"], inp["s5_c_im"],
                                                 inp["s5_a_re"], inp["s5_a_im"], inp["s5_log_dt"], inp["s5_d"])
            maps.append({"u": cc(proj[:, 128 * c:128 * (c + 1)]), "Bre": Bre, "Bim": Bim, "Cre": Cre, "Cim": Cim,
                         "par": par, "ramp": _RAMP, "ident": _IDENT})
        outs = _launch(_prog("s5", build_s5), maps)
        ysT = np.concatenate([o["ysT"] for o in outs], 0)
        maps = [{"pj": cc(proj[sl(i), 1024:2048]), "posT": cc(posT_all[:, i * NT:(i + 1) * NT]), "invf": invf,
                 "qa": inp["mla_q_a_norm"][l:l + 1], "kvn": inp["mla_kv_norm"][l:l + 1],
                 "gq": inp["mla_q_norm"][l:l + 1], "gk": inp["mla_k_norm"][l:l + 1],
                 "wq": inp["mla_w_q_b"][l], "wkv": inp["mla_w_kv_b"][l], "ident": _IDENT} for i in range(NCORE)]
        outs = _launch(_prog("front", build_mla_front), maps)
        qTo = np.concatenate([o["qTo"] for o in outs], 3)
        kTo = np.concatenate([o["kTo"] for o in outs], 3)
        vo = np.concatenate([o["vo"] for o in outs], 2)
        maps = []
        for b in range(NPAIR):
            qr = np.zeros((2, 128, SEQ), qTo.dtype)
            qr[0, 0:64] = qTo[b, 2, 0:64]
            qr[1, 64:128] = qTo[b, 2, 64:128]
            maps.append({"qn": cc(qTo[b, 0:2]), "qr": qr, "kn": cc(kTo[b, 0:2]), "kr": cc(kTo[b, 2]), "v": cc(vo[b]),
                         "pos": pos_row, "posT": posT_all, "ident": _IDENT})
        outs = _launch(_prog("attn", build_attn), maps)
        oT = np.concatenate([o["oT"] for o in outs], 0)
        common = lambda i: {"x": x[sl(i)], "ysT": cc(ysT[:, sl(i)]), "oT": cc(oT[:, sl(i)]), "pjq": cc(proj[sl(i), 2048:]),
                            "mem": mem, "mg": inp["mem_norm"][l:l + 1], "wglu": inp["s5_w_glu"][l], "wo": inp["mla_w_o"][l],
                            "wmkv": inp["mem_w_kv"][l], "mqn": inp["mem_q_norm"][l:l + 1], "mkn": inp["mem_k_norm"][l:l + 1],
                            "wmo": inp["mem_w_o"][l], "wout": inp["w_out"][l], "ident": _IDENT, "g": inp["norm_ffn"][l:l + 1]}
        if l % 2 == 0:
            maps = [dict(common(i), wgu=inp["ffn_w_gate_up"][l // 2:l // 2 + 1], wd=inp["ffn_w_down"][l // 2:l // 2 + 1])
                    for i in range(NCORE)]
            outs = _launch(_prog("mix_dense", build_mix, "dense"), maps)
            x = np.concatenate([o["y"] for o in outs], 0)
        else:
            rT = cc(inp["moe_router"][l // 2].T)
            maps = [dict(common(i), rT=rT, rb=inp["moe_router_b"][l // 2:l // 2 + 1]) for i in range(NCORE)]
            outs = _launch(_prog("mix_route", build_mix_route), maps)
            xmix = np.concatenate([o["y"] for o in outs], 0)
            hT = np.concatenate([o["hT"] for o in outs], 1)
            wts = np.concatenate([o["wts"] for o in outs], 0)
            maps = [{"hT": hT, "wc": cc(wts[:, e].reshape(SEQ // 128, 128).T), "wgu": inp["moe_w_gate_up"][l // 2, e],
                     "wd": inp["moe_w_down"][l // 2, e], "ident": _IDENT} for e in range(NEXP)]
            outs = _launch(_prog("moe", build_moe_expert), maps)
            parts = [o["part"] for o in outs]
            maps = [{"x": xmix[sl(i)], "parts": np.stack([p[sl(i)] for p in parts], 0)} for i in range(NCORE)]
            outs = _launch(_prog("combine", build_combine), maps)
            x = np.concatenate([o["y"] for o in outs], 0)
    return np.ascontiguousarray(x.astype(f32, copy=False)).reshape(1, SEQ, D)


ATT_SCALE = 192 ** -0.5


def phase_attn(P, C, qn_d, qr_d, kn_d, kr_d, v_d, pos_d, posT_d, oT_d):
    m = P.mark()
    NTK = SEQ // 128
    NQB = SEQ // 512
    QTn = [P.alloc_bf16(SEQ) for _ in range(2)]
    KTn = [P.alloc_bf16(SEQ) for _ in range(2)]
    QTr = [P.alloc_bf16(SEQ) for _ in range(2)]
    KTr = P.alloc_bf16(SEQ)
    V = [P.alloc_bf16(SEQ) for _ in range(2)]
    b_in = P.buf("attn_in")
    for hh in range(2):
        for half in range(2):
            sl = slice(half * 4096, (half + 1) * 4096)
            P.dma("pool", lambda e, hh=hh, sl=sl: e.dma_start(out=QTn[hh][:, sl], in_=qn_d.ap()[hh, :, sl]), writes=[b_in])
            P.dma("pool", lambda e, hh=hh, sl=sl: e.dma_start(out=KTn[hh][:, sl], in_=kn_d.ap()[hh, :, sl]), writes=[b_in])
            P.dma("pool", lambda e, hh=hh, half=half: e.dma_start(
                out=V[hh].rearrange("p (t d) -> p t d", d=128)[:, half * 32:(half + 1) * 32, :],
                in_=bass.AP(v_d, hh * SEQ * 128 + half * 4096 * 128, [[128, 128], [128 * 128, 32], [1, 128]])), writes=[b_in])
    for half in range(2):
        sl = slice(half * 4096, (half + 1) * 4096)
        for hh in range(2):
            P.dma("pool", lambda e, sl=sl, hh=hh: e.dma_start(out=QTr[hh][:, sl], in_=qr_d.ap()[hh, :, sl]), writes=[b_in])
        P.dma("pool", lambda e, sl=sl: e.dma_start(out=KTr[:, sl], in_=kr_d.ap()[:, sl]), writes=[b_in])
    NOPOS = DBG.get("attn_nopos", 0)
    pkf = P.alloc(64)
    posk = P.alloc_f32(64)
    b_pos = P.buf("pos")
    P.dma("sp", lambda e: e.dma_start(out=pkf, in_=posT_d.ap()), writes=[b_pos])
    P.op("dve", lambda e: e.tensor_copy(out=posk, in_=pkf.bitcast(I32)), reads=[b_pos], writes=[b_pos])
    pqi = [P.alloc(512) for _ in range(2)]
    b_pqi = [P.buf("pqi0"), P.buf("pqi1")]
    pq = [P.alloc_f32(512) for _ in range(2)]
    b_pq = [P.buf("pq0"), P.buf("pq1")]
    mk = [P.alloc_bf16(128) for _ in range(2)]
    b_mk = [P.buf("mk0"), P.buf("mk1")]
    pT = [P.alloc_bf16(512) for _ in range(3)]
    b_pT = [P.buf(f"pT{i}") for i in range(3)]
    rs = P.alloc_f32(512)
    b_rs = P.buf("rs")
    ost = OutStage(P, 512, 2, BF16)
    SB = [0, 1]
    OB = [2, 4]
    UB = [3, 5]
    P.ps_pool = [6, 7]
    blk = 0
    it = 0
    for hh in range(DBG.get('attn_heads', 2)):
        for Q in range(DBG.get('attn_nq', NQB)):
            s2 = blk % 2
            blk += 1
            P.dma("sp", lambda e, s2=s2, Q=Q: e.dma_start(out=pqi[s2], in_=bass.AP(pos_d, Q * 512, [[0, 128], [1, 512]])),
                  writes=[b_pqi[s2]])
            P.op("dve", lambda e, s2=s2: e.tensor_copy(out=pq[s2], in_=pqi[s2].bitcast(I32)), reads=[b_pqi[s2]], writes=[b_pq[s2]])
            pso, pbo = P.psum[OB[s2]], P.psb[OB[s2]]
            psu, pbu = P.psum[UB[s2]], P.psb[UB[s2]]
            nk = 4 * Q + 4

            def emit_s(j, it_j, hh=hh, Q=Q):
                pss, pbs = P.psum[SB[it_j % 2]], P.psb[SB[it_j % 2]]
                P.op("pe", lambda e: e.matmul(pss[:, :], KTn[hh][:, j * 128:(j + 1) * 128],
                                              QTn[hh][:, Q * 512:(Q + 1) * 512], start=True, stop=False),
                     reads=[b_in], writes=[pbs], signal=False)
                P.op("pe", lambda e: e.matmul(pss[:, :], KTr[:, j * 128:(j + 1) * 128],
                                              QTr[hh][:, Q * 512:(Q + 1) * 512], start=False, stop=True),
                     reads=[b_in], writes=[pbs], signal=True)
                c0 = 0 if j < 4 * Q else (j - 4 * Q) * 128
                return c0, pss, pbs

            pend = emit_s(0, it)
            for j in range(nk):
                c0, pss, pbs = pend
                s3 = it % 3
                it += 1
                if j + 1 < nk:
                    pend = emit_s(j + 1, it)
                P.op("act", lambda e, pss=pss, s3=s3: e.activation(out=pT[s3][:, :], in_=pss[:, :],
                                                                   func=AF.Exp, scale=ATT_SCALE),
                     reads=[pbs], writes=[b_pT[s3]])
                if c0 > 0:
                    P.op("dve", lambda e, c0=c0, s3=s3: e.memset(pT[s3][:, 0:c0], 0.0), reads=[b_pT[s3]], writes=[b_pT[s3]])
                if j >= 4 * Q and not NOPOS:
                    sm = j % 2
                    P.op("dve", lambda e, c0=c0, j=j, sm=sm, s2=s2: e.tensor_scalar(
                        out=mk[sm], in0=pq[s2][:, c0:c0 + 128], scalar1=posk[:, j:j + 1], scalar2=None, op0=ALU.is_ge),
                        reads=[b_pq[s2], b_pos], writes=[b_mk[sm]])
                    P.op("dve", lambda e, c0=c0, sm=sm, s3=s3: e.tensor_tensor(
                        out=pT[s3][:, c0:c0 + 128], in0=pT[s3][:, c0:c0 + 128], in1=mk[sm], op=ALU.mult),
                        reads=[b_mk[sm], b_pT[s3]], writes=[b_pT[s3]])
                P.op("pe", lambda e, j=j, s3=s3, pso=pso, hh=hh, nk=nk: e.matmul(
                    pso[:, :], V[hh][:, j * 128:(j + 1) * 128], pT[s3][:, :], start=(j == 0), stop=(j == nk - 1)),
                    reads=[b_in, b_pT[s3]], writes=[pbo] if (j == 0 or j == nk - 1) else [], signal=(j == nk - 1))
                P.op("pe", lambda e, j=j, s3=s3, psu=psu, nk=nk: e.matmul(
                    psu[:, :], C.onesb, pT[s3][:, :], start=(j == 0), stop=(j == nk - 1)),
                    reads=[C.b_ones, b_pT[s3]], writes=[pbu] if (j == 0 or j == nk - 1) else [], signal=(j == nk - 1))
            P.op("dve", lambda e, psu=psu: e.reciprocal(out=rs, in_=psu[:, :]), reads=[pbu], writes=[b_rs])
            o, bo = ost.next()
            P.op("dve", lambda e, pso=pso, o=o: e.tensor_tensor(out=o, in0=pso[:, :], in1=rs, op=ALU.mult),
                 reads=[pbo, b_rs], writes=[bo])
            P.dma("sp", lambda e, o=o, hh=hh, Q=Q: e.dma_start(out=oT_d.ap()[hh * 128:(hh + 1) * 128, Q * 512:(Q + 1) * 512], in_=o),
                  reads=[bo])
    P.ps_pool = list(range(8))
    P.release(m)


def build_attn():
    nc = bass.Bass("TRN2", target_bir_lowering=False)
    qn_d = nc.dram_tensor("qn", [2, 128, SEQ], BF16, kind="ExternalInput")
    qr_d = nc.dram_tensor("qr", [2, 128, SEQ], BF16, kind="ExternalInput")
    kn_d = nc.dram_tensor("kn", [2, 128, SEQ], BF16, kind="ExternalInput")
    kr_d = nc.dram_tensor("kr", [128, SEQ], BF16, kind="ExternalInput")
    v_d = nc.dram_tensor("v", [2, SEQ, 128], BF16, kind="ExternalInput")
    pos_d = nc.dram_tensor("pos", [1, SEQ], F32, kind="ExternalInput")
    posT_d = nc.dram_tensor("posT", [128, 64], F32, kind="ExternalInput")
    ident = nc.dram_tensor("ident", [128, 128], F32, kind="ExternalInput")
    oT_d = nc.dram_tensor("oT", [256, SEQ], BF16, kind="ExternalOutput")
    P = Prog(nc)
    C = Ctx(P, ident)
    phase_attn(P, C, qn_d, qr_d, kn_d, kr_d, v_d, pos_d, posT_d, oT_d)
    P.finish()
    return nc


NPAIR = 8


def emit_rope(P, C, src, rd, cs, sn, b_tab, dst, b_dst, tmp, b_tmp):
    x1, x2 = src[:, 0:32], src[:, 32:64]
    ta, tb = tmp[:, 0:32], tmp[:, 32:64]
    P.op("dve", lambda e: e.tensor_tensor(out=ta, in0=x1, in1=cs, op=ALU.mult), reads=rd + [b_tab], writes=[b_tmp])
    P.op("dve", lambda e: e.tensor_tensor(out=tb, in0=x2, in1=sn, op=ALU.mult), reads=rd + [b_tab], writes=[b_tmp])
    P.op("dve", lambda e: e.tensor_tensor(out=dst[:, 0:32], in0=ta, in1=tb, op=ALU.subtract), reads=[b_tmp], writes=[b_dst])
    P.op("dve", lambda e: e.tensor_tensor(out=ta, in0=x2, in1=cs, op=ALU.mult), reads=rd + [b_tab], writes=[b_tmp])
    P.op("dve", lambda e: e.tensor_tensor(out=tb, in0=x1, in1=sn, op=ALU.mult), reads=rd + [b_tab], writes=[b_tmp])
    P.op("dve", lambda e: e.tensor_tensor(out=dst[:, 32:64], in0=ta, in1=tb, op=ALU.add), reads=[b_tmp], writes=[b_dst])


def phase_mla_front(P, C, pj_d, posT_d, invf_d, qa_d, kvn_d, gq_d, gk_d, wq_d, wkv_d, qTo_d, kTo_d, vo_d, NTl=NT):
    m = P.mark()
    ntok = NTl * 128
    qa = P.alloc_f32(448); kvg = P.alloc_f32(512); gq = P.alloc_f32(192); gk = P.alloc_f32(192); invf = P.alloc_f32(32)
    b_gn = P.buf("gains")
    for dst, src, n in ((qa, qa_d, 448), (kvg, kvn_d, 512), (gq, gq_d, 192), (gk, gk_d, 192), (invf, invf_d, 32)):
        P.dma("sp", lambda e, dst=dst, src=src, n=n: e.dma_start(out=dst, in_=bc_row(src, 0, n)), writes=[b_gn])
    pkf = P.alloc(NTl)
    posc = P.alloc_f32(NTl)
    b_pos = P.buf("pos")
    P.dma("sp", lambda e: e.dma_start(out=pkf, in_=posT_d.ap()), writes=[b_pos])
    P.op("dve", lambda e: e.tensor_copy(out=posc, in_=pkf.bitcast(I32)), reads=[b_pos], writes=[b_pos])
    na = NTl * 32
    ang = P.alloc_f32(na); cosT = P.alloc_f32(na); sinT = P.alloc_f32(na)
    b_ang = P.buf("ang"); b_tab = P.buf("ropetab")
    sc_tmp = P.alloc_f32(2 * na); sc_ti = P.alloc(na).bitcast(I32); b_sc = P.buf("sc")
    for t in range(NTl):
        P.op("dve", lambda e, t=t: e.tensor_scalar(out=ang[:, t * 32:(t + 1) * 32], in0=invf, scalar1=posc[:, t:t + 1],
                                                  scalar2=None, op0=ALU.mult), reads=[b_gn, b_pos], writes=[b_ang])
    emit_sincos(P, C, ang, b_ang, na, sinT, cosT, b_tab, sc_tmp, sc_ti, b_sc)
    cqT = P.alloc_bf16(4 * ntok); cqT3 = cqT.rearrange("p (k t) -> p k t", k=4)
    ckT = P.alloc_bf16(4 * ntok); ckT3 = ckT.rearrange("p (k t) -> p k t", k=4)
    b_cq = [P.buf(f"cq{t}") for t in range(NTl)]
    b_ck = [P.buf(f"ck{t}") for t in range(NTl)]
    krall = P.alloc_f32(NTl * 64); krss = P.alloc_f32(NTl)
    b_kr = [P.buf(f"kr{t}") for t in range(NTl)]
    pjt = [P.alloc_bf16(1024) for _ in range(2)]; b_pj = [P.buf("pj0"), P.buf("pj1")]
    cqn = [P.alloc_bf16(448) for _ in range(2)]; b_cqn = [P.buf("cqn0"), P.buf("cqn1")]
    ckn = [P.alloc_bf16(512) for _ in range(2)]; b_ckn = [P.buf("ckn0"), P.buf("ckn1")]
    for t in range(NTl):
        s = t % 2
        P.dma("sp", lambda e, t=t, s=s: e.dma_start(out=pjt[s], in_=pj_d.ap()[t * 128:(t + 1) * 128, :]), writes=[b_pj[s]])
        emit_rmsnorm(P, C, pjt[s][:, 0:448], [b_pj[s]], qa, b_gn, cqn[s], b_cqn[s], 448)
        emit_rmsnorm(P, C, pjt[s][:, 448:960], [b_pj[s]], kvg, b_gn, ckn[s], b_ckn[s], 512)
        emit_transposes(P, C, cqn[s], b_cqn[s], [128, 128, 128, 64],
                        lambda j, t=t: cqT3[0:(64 if j == 3 else 128), j, t * 128:(t + 1) * 128], b_cq[t])
        emit_transposes(P, C, ckn[s], b_ckn[s], [128] * 4, lambda j, t=t: ckT3[:, j, t * 128:(t + 1) * 128], b_ck[t])
        P.op("act", lambda e, t=t, s=s: e.activation(out=krall[:, t * 64:(t + 1) * 64], in_=pjt[s][:, 960:1024], func=AF.Copy),
             reads=[b_pj[s]], writes=[b_kr[t]])
        jk, b_jk = C.next_junk()
        P.op("act", lambda e, t=t, s=s, jk=jk: e.activation(out=jk[:, 0:64], in_=pjt[s][:, 960:1024], func=AF.Square,
                                                            accum_out=krss[:, t:t + 1]), reads=[b_pj[s]], writes=[b_kr[t], b_jk])
    NST = 2
    stq = [P.alloc_bf16(384) for _ in range(NST)]; b_stq = [P.buf(f"stq{i}") for i in range(NST)]
    qf = [P.alloc_f32(192) for _ in range(NST)]; b_qf = [P.buf(f"qf{i}") for i in range(NST)]
    rtmp = P.alloc_f32(64); b_rtmp = P.buf("rtmp")
    qTs = [P.alloc_bf16(384) for _ in range(NST)]; b_qTs = [P.buf(f"qTs{i}") for i in range(NST)]
    cnt = [0]

    def epi_q(t, b, ps, pb):
        si = cnt[0] % NST
        cnt[0] += 1
        for hh in range(2):
            src = ps[:, hh * 192:(hh + 1) * 192]
            ss, b_ss = C.next_ss()
            jk, b_jk = C.next_junk()
            P.op("act", lambda e, src=src, ss=ss, jk=jk: e.activation(out=jk[:, 0:192], in_=src, func=AF.Square, accum_out=ss[:, 0:1]),
                 reads=[pb], writes=[b_ss, b_jk])
            emit_rstd(P, C, ss, b_ss, 192)
            P.op("dve", lambda e, src=src, ss=ss, si=si: e.scalar_tensor_tensor(out=qf[si], in0=src, scalar=ss[:, 1:2], in1=gq,
                                                                                  op0=ALU.mult, op1=ALU.mult),
                 reads=[pb, b_ss, b_gn], writes=[b_qf[si]])
            P.op("act", lambda e, si=si, hh=hh: e.activation(out=stq[si][:, hh * 128:(hh + 1) * 128], in_=qf[si][:, 0:128], func=AF.Copy),
                 reads=[b_qf[si]], writes=[b_stq[si]])
            emit_rope(P, C, qf[si][:, 128:192], [b_qf[si]], cosT[:, t * 32:(t + 1) * 32], sinT[:, t * 32:(t + 1) * 32], b_tab,
                      stq[si][:, 256 + hh * 64:256 + (hh + 1) * 64], b_stq[si], rtmp, b_rtmp)
        q3 = qTs[si].rearrange("p (k t) -> p k t", k=3)
        emit_transposes(P, C, stq[si], b_stq[si], [128] * 3, lambda j, q3=q3: q3[:, j, :], b_qTs[si])
        P.dma("sp", lambda e, q3=q3, b=b, t=t: e.dma_start(
            out=bass.AP(qTo_d, b * 3 * 128 * ntok + t * 128, [[ntok, 128], [128 * ntok, 3], [1, 128]]), in_=q3), reads=[b_qTs[si]])

    linear_tm(P, C, cqT3, b_cq, [128, 128, 128, 64], NTl, wq_d, 0, 3072, [b * 384 for b in range(NPAIR)], 384, epi_q)
    stk = [P.alloc_bf16(384) for _ in range(NST)]; b_stk = [P.buf(f"stk{i}") for i in range(NST)]
    krn = [P.alloc_f32(64) for _ in range(NST)]; b_krn = [P.buf(f"krn{i}") for i in range(NST)]
    kTs = [P.alloc_bf16(384) for _ in range(NST)]; b_kTs = [P.buf(f"kTs{i}") for i in range(NST)]
    vst = [P.alloc_bf16(256) for _ in range(NST)]; b_vst = [P.buf(f"vst{i}") for i in range(NST)]

    def epi_kv(t, b, ps, pb):
        si = cnt[0] % NST
        cnt[0] += 1
        for hh in range(2):
            ksrc = ps[:, hh * 256:hh * 256 + 128]
            vsrc = ps[:, hh * 256 + 128:hh * 256 + 256]
            ss, b_ss = C.next_ss()
            jk, b_jk = C.next_junk()
            P.op("act", lambda e, ksrc=ksrc, ss=ss, jk=jk: e.activation(out=jk[:, 0:128], in_=ksrc, func=AF.Square, accum_out=ss[:, 0:1]),
                 reads=[pb], writes=[b_ss, b_jk])
            P.op("dve", lambda e, ss=ss, t=t: e.tensor_tensor(out=ss[:, 0:1], in0=ss[:, 0:1], in1=krss[:, t:t + 1], op=ALU.add),
                 reads=[b_ss, b_kr[t]], writes=[b_ss])
            emit_rstd(P, C, ss, b_ss, 192)
            P.op("dve", lambda e, ksrc=ksrc, ss=ss, si=si, hh=hh: e.scalar_tensor_tensor(
                out=stk[si][:, hh * 128:(hh + 1) * 128], in0=ksrc, scalar=ss[:, 1:2], in1=gk[:, 0:128], op0=ALU.mult, op1=ALU.mult),
                reads=[pb, b_ss, b_gn], writes=[b_stk[si]])
            P.op("dve", lambda e, ss=ss, si=si, t=t: e.scalar_tensor_tensor(
                out=krn[si], in0=krall[:, t * 64:(t + 1) * 64], scalar=ss[:, 1:2], in1=gk[:, 128:192], op0=ALU.mult, op1=ALU.mult),
                reads=[b_kr[t], b_ss, b_gn], writes=[b_krn[si]])
            emit_rope(P, C, krn[si], [b_krn[si]], cosT[:, t * 32:(t + 1) * 32], sinT[:, t * 32:(t + 1) * 32], b_tab,
                      stk[si][:, 256 + hh * 64:256 + (hh + 1) * 64], b_stk[si], rtmp, b_rtmp)
            P.op("act", lambda e, vsrc=vsrc, si=si, hh=hh: e.activation(out=vst[si][:, hh * 128:(hh + 1) * 128], in_=vsrc, func=AF.Copy),
                 reads=[pb], writes=[b_vst[si]])
        k3 = kTs[si].rearrange("p (k t) -> p k t", k=3)
        emit_transposes(P, C, stk[si], b_stk[si], [128] * 3, lambda j, k3=k3: k3[:, j, :], b_kTs[si])
        P.dma("sp", lambda e, k3=k3, b=b, t=t: e.dma_start(
            out=bass.AP(kTo_d, b * 3 * 128 * ntok + t * 128, [[ntok, 128], [128 * ntok, 3], [1, 128]]), in_=k3), reads=[b_kTs[si]])
        P.dma("act", lambda e, si=si, b=b, t=t: e.dma_start(
            out=bass.AP(vo_d, b * 2 * ntok * 128 + t * 128 * 128, [[128, 128], [ntok * 128, 2], [1, 128]]),
            in_=vst[si].rearrange("p (h d) -> p h d", h=2)), reads=[b_vst[si]])

    linear_tm(P, C, ckT3, b_ck, [128] * 4, NTl, wkv_d, 0, 4096, [b * 512 for b in range(NPAIR)], 512, epi_kv)
    P.release(m)


def build_mla_front(NTl=NT):
    nc = bass.Bass("TRN2", target_bir_lowering=False)
    ntok = NTl * 128
    pj_d = nc.dram_tensor("pj", [ntok, 1024], BF16, kind="ExternalInput")
    posT_d = nc.dram_tensor("posT", [128, NTl], F32, kind="ExternalInput")
    invf_d = nc.dram_tensor("invf", [1, 32], F32, kind="ExternalInput")
    qa_d = nc.dram_tensor("qa", [1, 448], F32, kind="ExternalInput")
    kvn_d = nc.dram_tensor("kvn", [1, 512], F32, kind="ExternalInput")
    gq_d = nc.dram_tensor("gq", [1, 192], F32, kind="ExternalInput")
    gk_d = nc.dram_tensor("gk", [1, 192], F32, kind="ExternalInput")
    wq_d = nc.dram_tensor("wq", [448, 3072], F32, kind="ExternalInput")
    wkv_d = nc.dram_tensor("wkv", [512, 4096], F32, kind="ExternalInput")
    ident = nc.dram_tensor("ident", [128, 128], F32, kind="ExternalInput")
    qTo_d = nc.dram_tensor("qTo", [NPAIR, 3, 128, ntok], BF16, kind="ExternalOutput")
    kTo_d = nc.dram_tensor("kTo", [NPAIR, 3, 128, ntok], BF16, kind="ExternalOutput")
    vo_d = nc.dram_tensor("vo", [NPAIR, 2, ntok, 128], BF16, kind="ExternalOutput")
    P = Prog(nc)
    C = Ctx(P, ident)
    phase_mla_front(P, C, pj_d, posT_d, invf_d, qa_d, kvn_d, gq_d, gk_d, wq_d, wkv_d, qTo_d, kTo_d, vo_d, NTl)
    P.finish()
    return nc


MEMN = 256


def phase_mix(P, C, x_d, ysT_d, oT_d, pjq_d, mem_d, mg_d, wglu_d, wo_d, wmkv_d, mqn_d, mkn_d, wmo_d, wout_d, macc, b_macc):
    GOFF = 1024

    class G:
        gl = [P.alloc_bf16(512) for _ in range(3)]
        b_gl = [P.buf(f"gl{i}") for i in range(3)]
        gs = [P.alloc_f32(512) for _ in range(3)]
        b_gs = [P.buf(f"gs{i}") for i in range(3)]
        tmp = [P.alloc_f32(512) for _ in range(3)]
        b_tmp = [P.buf(f"gtmp{i}") for i in range(3)]
        i = 0

    def gate_sig(t, nb, br):
        i = G.i % 3
        G.i += 1
        c0 = GOFF + br * D + nb * 512
        P.dma("sp", lambda e: e.dma_start(out=G.gl[i], in_=pjq_d.ap()[t * 128:(t + 1) * 128, c0:c0 + 512]), writes=[G.b_gl[i]])
        P.op("act", lambda e: e.activation(out=G.gs[i], in_=G.gl[i], func=AF.Sigmoid), reads=[G.b_gl[i]], writes=[G.b_gs[i]])
        return i

    def epi_acc(br):
        def f(t, nb, ps, pb):
            i = gate_sig(t, nb, br)
            ma = macc[t][:, nb * 512:(nb + 1) * 512]
            P.op("dve", lambda e: e.tensor_tensor(out=G.tmp[i], in0=ps[:, :], in1=G.gs[i], op=ALU.mult),
                 reads=[pb, G.b_gs[i]], writes=[G.b_tmp[i]])
            P.op("dve", lambda e: e.tensor_tensor(out=ma, in0=ma, in1=G.tmp[i], op=ALU.add),
                 reads=[G.b_tmp[i], b_macc[t]], writes=[b_macc[t]])
        return f

    blocks = [0, 512, 1024, 1536]
    m1 = P.mark()
    ys = P.alloc_bf16(8 * TPC); ys3 = ys.rearrange("p (k t) -> p k t", k=8); b_ys = P.buf("ys")
    P.dma("sp", lambda e: e.dma_start(out=ys3, in_=bass.AP(ysT_d, 0, [[TPC, 128], [128 * TPC, 8], [1, TPC]])), writes=[b_ys])

    def epi_glu(t, nb, psA, pbA, psB, pbB):
        i = gate_sig(t, nb, 0)
        ma = macc[t][:, nb * 512:(nb + 1) * 512]
        P.op("act", lambda e: e.activation(out=G.tmp[i], in_=psB[:, :], func=AF.Sigmoid), reads=[pbB], writes=[G.b_tmp[i]])
        P.op("dve", lambda e: e.tensor_tensor(out=G.tmp[i], in0=psA[:, :], in1=G.tmp[i], op=ALU.mult),
             reads=[pbA, G.b_tmp[i]], writes=[G.b_tmp[i]])
        P.op("dve", lambda e: e.tensor_tensor(out=ma, in0=G.tmp[i], in1=G.gs[i], op=ALU.mult),
             reads=[G.b_tmp[i], G.b_gs[i]], writes=[b_macc[t]])

    linear_tm(P, C, ys3, [b_ys] * NT, [128] * 8, NT, wglu_d, 0, 4096, blocks, 512, epi_glu, pair_off=2048)
    P.release(m1)
    o = P.alloc_bf16(16 * TPC); o3 = o.rearrange("p (k t) -> p k t", k=16); b_o = P.buf("oT")
    for half in range(2):
        P.dma("sp", lambda e, half=half: e.dma_start(out=o3[:, half * 8:(half + 1) * 8, :],
                                                      in_=bass.AP(oT_d, half * 8 * 128 * TPC, [[TPC, 128], [128 * TPC, 8], [1, TPC]])),
              writes=[b_o])
    linear_tm(P, C, o3, [b_o] * NT, [128] * 16, NT, wo_d, 0, D, blocks, 512, epi_acc(1))
    P.release(m1)
    gbc = P.alloc_f32(D); b_g = P.buf("memg")
    P.dma("sp", lambda e: e.dma_start(out=gbc, in_=bc_row(mg_d, 0, D)), writes=[b_g])
    qg = P.alloc_f32(256); kg = P.alloc_f32(256); b_qkg = P.buf("qkg")
    P.dma("sp", lambda e: e.dma_start(out=qg, in_=bc_row(mqn_d, 0, 256)), writes=[b_qkg])
    P.dma("sp", lambda e: e.dma_start(out=kg, in_=bc_row(mkn_d, 0, 256)), writes=[b_qkg])
    mT = P.alloc_bf16(KC_D * MEMN); mT3 = mT.rearrange("p (k t) -> p k t", k=KC_D)
    b_mT = [P.buf("mT0"), P.buf("mT1")]
    knT = P.alloc_bf16(8 * MEMN); knT3 = knT.rearrange("p (k t) -> p k t", k=8); b_knT = P.buf("knT")
    vm = [P.alloc_bf16(1024) for _ in range(2)]; b_vm = [P.buf("vm0"), P.buf("vm1")]
    m3 = P.mark()
    mt_ = [P.alloc_f32(D) for _ in range(2)]; b_mt = [P.buf("mt0"), P.buf("mt1")]

    def mem_src(t):
        P.dma("sp", lambda e: e.dma_start(out=mt_[t], in_=mem_d.ap()[t * 128:(t + 1) * 128, :]), writes=[b_mt[t]])
        return mt_[t], [b_mt[t]]

    norm_transpose(P, C, 2, mem_src, gbc, b_g, mT3, b_mT)
    kst = [P.alloc_bf16(512) for _ in range(2)]; b_kst = [P.buf("kst0"), P.buf("kst1")]
    kc = [0]

    def epi_memkv(t, nb, ps, pb):
        if nb < 2:
            si = kc[0] % 2
            kc[0] += 1
            for hh in range(2):
                emit_rmsnorm(P, C, ps[:, hh * 256:(hh + 1) * 256], [pb], kg, b_qkg, kst[si][:, hh * 256:(hh + 1) * 256], b_kst[si], 256)
            emit_transposes(P, C, kst[si], b_kst[si], [128] * 4,
                            lambda j: knT3[:, nb * 4 + j, t * 128:(t + 1) * 128], b_knT)
        else:
            C.evac(vm[t][:, (nb - 2) * 512:(nb - 1) * 512], ps[:, :], [pb], [b_vm[t]])

    linear_tm(P, C, mT3, b_mT, [128] * KC_D, 2, wmkv_d, 0, D, blocks, 512, epi_memkv)
    P.release(m3)
    qnT = P.alloc_bf16(8 * TPC); qnT3 = qnT.rearrange("p (k t) -> p k t", k=8)
    b_qnT = [P.buf(f"qnT{t}") for t in range(NT)]
    omT = P.alloc_bf16(8 * TPC); omT3 = omT.rearrange("p (k t) -> p k t", k=8)
    b_omT = [P.buf(f"omT{tb}") for tb in range(TPC // 512)]
    m3 = P.mark()
    qt = [P.alloc_bf16(1024) for _ in range(2)]; b_qt = [P.buf("qt0"), P.buf("qt1")]
    qnb = [P.alloc_bf16(1024) for _ in range(2)]; b_qnb = [P.buf("qnb0"), P.buf("qnb1")]
    for t in range(NT):
        s = t % 2
        P.dma("sp", lambda e, t=t, s=s: e.dma_start(out=qt[s], in_=pjq_d.ap()[t * 128:(t + 1) * 128, 0:1024]), writes=[b_qt[s]])
        for h in range(4):
            emit_rmsnorm(P, C, qt[s][:, h * 256:(h + 1) * 256], [b_qt[s]], qg, b_qkg, qnb[s][:, h * 256:(h + 1) * 256], b_qnb[s], 256)
        emit_transposes(P, C, qnb[s], b_qnb[s], [128] * 8, lambda j, t=t: qnT3[:, j, t * 128:(t + 1) * 128], b_qnT[t])
    pTm = [P.alloc_bf16(512) for _ in range(4)]; b_pTm = [P.buf(f"pTm{i}") for i in range(4)]
    rsm = P.alloc_f32(512); b_rsm = P.buf("rsm")
    pi_ = 0
    for tb in range(TPC // 512):
        for h in range(4):
            sl = []
            for mt in range(2):
                ps, pb = P.next_psum()
                for dc in range(2):
                    P.op("pe", lambda e, ps=ps, h=h, dc=dc, mt=mt, tb=tb: e.matmul(
                        ps[:, :], knT3[:, 2 * h + dc, mt * 128:(mt + 1) * 128], qnT3[:, 2 * h + dc, tb * 512:(tb + 1) * 512],
                        start=(dc == 0), stop=(dc == 1)),
                        reads=[b_knT] + b_qnT[tb * 4:(tb + 1) * 4], writes=[pb], signal=(dc == 1))
                i = pi_ % 4
                pi_ += 1
                P.op("act", lambda e, ps=ps, i=i: e.activation(out=pTm[i], in_=ps[:, :], func=AF.Exp, scale=1.0 / 16.0),
                     reads=[pb], writes=[b_pTm[i]])
                sl.append(i)
            psu, pbu = P.next_psum()
            for mt in range(2):
                P.op("pe", lambda e, psu=psu, mt=mt, i=sl[mt]: e.matmul(psu[:, :], C.onesb, pTm[i], start=(mt == 0), stop=(mt == 1)),
                     reads=[C.b_ones, b_pTm[sl[mt]]], writes=[pbu], signal=(mt == 1))
            P.op("dve", lambda e, psu=psu: e.reciprocal(out=rsm, in_=psu[:, :]), reads=[pbu], writes=[b_rsm])
            for dc in range(2):
                pso, pbo = P.next_psum()
                for mt in range(2):
                    P.op("pe", lambda e, pso=pso, mt=mt, i=sl[mt], h=h, dc=dc: e.matmul(
                        pso[:, :], vm[mt][:, h * 256 + dc * 128:h * 256 + (dc + 1) * 128], pTm[i], start=(mt == 0), stop=(mt == 1)),
                        reads=[b_vm[mt], b_pTm[sl[mt]]], writes=[pbo], signal=(mt == 1))
                P.op("dve", lambda e, pso=pso, h=h, dc=dc, tb=tb: e.tensor_tensor(
                    out=omT3[:, 2 * h + dc, tb * 512:(tb + 1) * 512], in0=pso[:, :], in1=rsm, op=ALU.mult),
                    reads=[pbo, b_rsm], writes=[b_omT[tb]])
    P.release(m3)
    linear_tm(P, C, omT3, [b_omT[t // 4] for t in range(NT)], [128] * 8, NT, wmo_d, 0, D, blocks, 512, epi_acc(2))
    P.release(m1)
    mg = P.alloc_bf16(KC_D * TPC); mg3 = mg.rearrange("p (k t) -> p k t", k=KC_D)
    b_mg = [P.buf(f"mg{t}") for t in range(NT)]
    hb = [P.alloc_bf16(D) for _ in range(2)]; b_hb = [P.buf("mhb0"), P.buf("mhb1")]
    for t in range(NT):
        s = t % 2
        P.op("act", lambda e, t=t, s=s: e.activation(out=hb[s], in_=macc[t], func=AF.Copy), reads=[b_macc[t]], writes=[b_hb[s]])
        emit_transposes(P, C, hb[s], b_hb[s], [128] * KC_D, lambda j, t=t: mg3[:, j, t * 128:(t + 1) * 128], b_mg[t])
        P.dma("sp", lambda e, t=t: e.dma_start(out=macc[t], in_=x_d.ap()[t * 128:(t + 1) * 128, :]), writes=[b_macc[t]])

    def epi_res(t, nb, ps, pb):
        ma = macc[t][:, nb * 512:(nb + 1) * 512]
        P.op("dve", lambda e: e.tensor_tensor(out=ma, in0=ps[:, :], in1=ma, op=ALU.add), reads=[pb, b_macc[t]], writes=[b_macc[t]])

    linear_tm(P, C, mg3, b_mg, [128] * KC_D, NT, wout_d, 0, D, blocks, 512, epi_res)
    P.release(m1)


def build_mix(ffn):
    nc = bass.Bass("TRN2", target_bir_lowering=False)
    dt_ = nc.dram_tensor
    x_d = dt_("x", [TPC, D], F32, kind="ExternalInput")
    ysT_d = dt_("ysT", [1024, TPC], BF16, kind="ExternalInput")
    oT_d = dt_("oT", [D, TPC], BF16, kind="ExternalInput")
    pjq_d = dt_("pjq", [TPC, 7168], BF16, kind="ExternalInput")
    mem_d = dt_("mem", [MEMN, D], F32, kind="ExternalInput")
    mg_d = dt_("mg", [1, D], F32, kind="ExternalInput")
    wglu_d = dt_("wglu", [1024, 4096], F32, kind="ExternalInput")
    wo_d = dt_("wo", [D, D], F32, kind="ExternalInput")
    wmkv_d = dt_("wmkv", [D, D], F32, kind="ExternalInput")
    mqn_d = dt_("mqn", [1, 256], F32, kind="ExternalInput")
    mkn_d = dt_("mkn", [1, 256], F32, kind="ExternalInput")
    wmo_d = dt_("wmo", [1024, D], F32, kind="ExternalInput")
    wout_d = dt_("wout", [D, D], F32, kind="ExternalInput")
    ident = dt_("ident", [128, 128], F32, kind="ExternalInput")
    if ffn == "dense":
        g_d = dt_("g", [1, D], F32, kind="ExternalInput")
        wgu_d = dt_("wgu", [1, D, 2 * DFF], F32, kind="ExternalInput")
        wd_d = dt_("wd", [1, DFF, D], F32, kind="ExternalInput")
    y_d = dt_("y", [TPC, D], F32, kind="ExternalOutput")
    P = Prog(nc)
    C = Ctx(P, ident)
    macc = [P.alloc_f32(D) for _ in range(NT)]
    b_macc = [P.buf(f"macc{t}") for t in range(NT)]
    phase_mix(P, C, x_d, ysT_d, oT_d, pjq_d, mem_d, mg_d, wglu_d, wo_d, wmkv_d, mqn_d, mkn_d, wmo_d, wout_d, macc, b_macc)
    if ffn == "dense":
        phase_ffn(P, C, macc, b_macc, g_d, 0, [(wgu_d, 0)], [(wd_d, 0)], None)
    for t in range(NT):
        P.dma("sp", lambda e, t=t: e.dma_start(out=y_d.ap()[t * 128:(t + 1) * 128, :], in_=macc[t]), reads=[b_macc[t]])
    P.finish()
    return nc


def build_mix_route():
    nc = bass.Bass("TRN2", target_bir_lowering=False)
    dt_ = nc.dram_tensor
    x_d = dt_("x", [TPC, D], F32, kind="ExternalInput")
    ysT_d = dt_("ysT", [1024, TPC], BF16, kind="ExternalInput")
    oT_d = dt_("oT", [D, TPC], BF16, kind="ExternalInput")
    pjq_d = dt_("pjq", [TPC, 7168], BF16, kind="ExternalInput")
    mem_d = dt_("mem", [MEMN, D], F32, kind="ExternalInput")
    mg_d = dt_("mg", [1, D], F32, kind="ExternalInput")
    wglu_d = dt_("wglu", [1024, 4096], F32, kind="ExternalInput")
    wo_d = dt_("wo", [D, D], F32, kind="ExternalInput")
    wmkv_d = dt_("wmkv", [D, D], F32, kind="ExternalInput")
    mqn_d = dt_("mqn", [1, 256], F32, kind="ExternalInput")
    mkn_d = dt_("mkn", [1, 256], F32, kind="ExternalInput")
    wmo_d = dt_("wmo", [1024, D], F32, kind="ExternalInput")
    wout_d = dt_("wout", [D, D], F32, kind="ExternalInput")
    ident = dt_("ident", [128, 128], F32, kind="ExternalInput")
    g_d = dt_("g", [1, D], F32, kind="ExternalInput")
    rT_d = dt_("rT", [NEXP, D], F32, kind="ExternalInput")
    rb_d = dt_("rb", [1, NEXP], F32, kind="ExternalInput")
    y_d = dt_("y", [TPC, D], F32, kind="ExternalOutput")
    hT_d = dt_("hT", [D, TPC], BF16, kind="ExternalOutput")
    wts_d = dt_("wts", [TPC, NEXP], F32, kind="ExternalOutput")
    P = Prog(nc)
    C = Ctx(P, ident)
    macc = [P.alloc_f32(D) for _ in range(NT)]
    b_macc = [P.buf(f"macc{t}") for t in range(NT)]
    phase_mix(P, C, x_d, ysT_d, oT_d, pjq_d, mem_d, mg_d, wglu_d, wo_d, wmkv_d, mqn_d, mkn_d, wmo_d, wout_d, macc, b_macc)
    phase_ffn(P, C, macc, b_macc, g_d, 0, [None] * NEXP, [None] * NEXP, router=(rT_d, 0, rb_d, 0), route_out=(hT_d, wts_d))
    for t in range(NT):
        P.dma("sp", lambda e, t=t: e.dma_start(out=y_d.ap()[t * 128:(t + 1) * 128, :], in_=macc[t]), reads=[b_macc[t]])
    P.finish()
    return nc


def build_moe_expert(nblk=SEQ // TPC):
    nc = bass.Bass("TRN2", target_bir_lowering=False)
    ntok = nblk * TPC
    hT_d = nc.dram_tensor("hT", [D, ntok], BF16, kind="ExternalInput")
    wc_d = nc.dram_tensor("wc", [128, ntok // 128], F32, kind="ExternalInput")
    wgu_d = nc.dram_tensor("wgu", [D, 2 * DFF], F32, kind="ExternalInput")
    wd_d = nc.dram_tensor("wd", [DFF, D], F32, kind="ExternalInput")
    ident = nc.dram_tensor("ident", [128, 128], F32, kind="ExternalInput")
    part_d = nc.dram_tensor("part", [ntok, D], BF16, kind="ExternalOutput")
    P = Prog(nc)
    C = Ctx(P, ident)
    wc = P.alloc_f32(ntok // 128)
    b_wc = P.buf("wc")
    P.dma("sp", lambda e: e.dma_start(out=wc, in_=wc_d.ap()), writes=[b_wc])
    acc = [P.alloc_f32(D) for _ in range(NT)]
    b_acc = [P.buf(f"acc{t}") for t in range(NT)]
    hT = P.alloc_bf16(KC_D * TPC)
    hT3 = hT.rearrange("p (k t) -> p k t", k=KC_D)
    b_hT = [P.buf(f"hT{t}") for t in range(NT)]
    ost = OutStage(P, D, 2, BF16)
    for blk in range(nblk):
        for half in range(2):
            P.dma("sp", lambda e, half=half, blk=blk: e.dma_start(
                out=hT3[:, half * 8:(half + 1) * 8, :],
                in_=bass.AP(hT_d, half * 8 * 128 * ntok + blk * TPC, [[ntok, 128], [128 * ntok, 8], [1, TPC]])), writes=b_hT)
        ffn_body(P, C, hT3, b_hT, acc, b_acc, [(wgu_d, 0)], [(wd_d, 0)],
                 wcol_fn=lambda t, ex, blk=blk: (wc[:, blk * NT + t:blk * NT + t + 1], b_wc), init_zero=True)
        for t in range(NT):
            o, bo = ost.next()
            P.op("act", lambda e, o=o, t=t: e.activation(out=o, in_=acc[t], func=AF.Copy), reads=[b_acc[t]], writes=[bo])
            P.dma("sp", lambda e, o=o, t=t, blk=blk: e.dma_start(out=part_d.ap()[blk * TPC + t * 128:blk * TPC + (t + 1) * 128, :], in_=o),
                  reads=[bo])
    P.finish()
    return nc


def build_combine():
    nc = bass.Bass("TRN2", target_bir_lowering=False)
    x_d = nc.dram_tensor("x", [TPC, D], F32, kind="ExternalInput")
    parts_d = nc.dram_tensor("parts", [NEXP, TPC, D], BF16, kind="ExternalInput")
    y_d = nc.dram_tensor("y", [TPC, D], F32, kind="ExternalOutput")
    P = Prog(nc)
    xt = [P.alloc_f32(D) for _ in range(2)]
    b_x = [P.buf("cx0"), P.buf("cx1")]
    pt = [P.alloc_bf16(D) for _ in range(3)]
    b_p = [P.buf(f"cp{i}") for i in range(3)]
    k = 0
    for t in range(NT):
        s = t % 2
        P.dma("sp", lambda e, t=t, s=s: e.dma_start(out=xt[s], in_=x_d.ap()[t * 128:(t + 1) * 128, :]), writes=[b_x[s]])
        for ex in range(NEXP):
            i = k % 3
            k += 1
            P.dma("act" if ex % 2 else "sp", lambda e, t=t, ex=ex, i=i: e.dma_start(out=pt[i], in_=parts_d.ap()[ex, t * 128:(t + 1) * 128, :]),
                  writes=[b_p[i]])
            P.op("dve", lambda e, s=s, i=i: e.tensor_tensor(out=xt[s], in0=xt[s], in1=pt[i], op=ALU.add),
                 reads=[b_p[i], b_x[s]], writes=[b_x[s]])
        P.dma("sp", lambda e, t=t, s=s: e.dma_start(out=y_d.ap()[t * 128:(t + 1) * 128, :], in_=xt[s]), reads=[b_x[s]])
    P.finish()
    return nc
```
